# Optimizing a Trainium2 kernel written in Bass

```python
import jax, jax.numpy as jnp
from jax import lax
import numpy as np

D_MODEL = 2048
BATCH = 1
SEQ = 16384
DEPTH = 2

N_HEADS = 16
HEAD_DIM = D_MODEL // N_HEADS
N_KV_GROUPS = 4
HEADS_PER_GROUP = N_HEADS // N_KV_GROUPS
KV_WIDTH = N_KV_GROUPS * HEAD_DIM
CMP_BLOCK = 32
CMP_STRIDE = 16
SLC_BLOCK = 64
RATIO = SLC_BLOCK // CMP_STRIDE
N_SLC = 16
WINDOW = 512
Q_BLOCK = 128
CMP_HIDDEN = 2 * HEAD_DIM
ROPE_THETA = 10000.0
NSA_IN_WIDTH = N_HEADS * HEAD_DIM + 6 * KV_WIDTH + 3 * N_HEADS

POOL_WINDOWS = (2, 4, 8, 16)
N_POOL_GROUPS = 4
POOL_GROUP = D_MODEL // N_POOL_GROUPS

MEM_LEN = 256
MEM_HEADS = 4
MEM_HEAD_DIM = 128
MEM_WIDTH = MEM_HEADS * MEM_HEAD_DIM

D_FF = 5632
N_EXPERTS = 8
TOP_K = 2
MOE_BLOCK = 512

N_MIXER_A_LAYERS = (DEPTH + 1) // 2
N_MIXER_B_LAYERS = DEPTH // 2
EPS = 1e-6
NEG_INF = -1e30
FORCE = 1e9

kernel_name = "hybrid_nsa_pool_moe_trunk"


def rmsnorm(x, g):
    xf = x.astype(jnp.float32)
    r = lax.rsqrt(jnp.mean(xf * xf, axis=-1, keepdims=True) + EPS)
    return (xf * r).astype(x.dtype) * g


def rope(x, pos):
    half = x.shape[-1] // 2
    inv = ROPE_THETA ** (-jnp.arange(half, dtype=jnp.float32) / half)
    ang = pos.astype(jnp.float32)[:, None] * inv[None, :]
    shape = (1, pos.shape[0]) + (1,) * (x.ndim - 3) + (half,)
    cos = jnp.cos(ang).reshape(shape).astype(x.dtype)
    sin = jnp.sin(ang).reshape(shape).astype(x.dtype)
    x1, x2 = x[..., :half], x[..., half:]
    return jnp.concatenate([x1 * cos - x2 * sin, x2 * cos + x1 * sin], axis=-1)


def masked_softmax(s, mask):
    p = jax.nn.softmax(jnp.where(mask, s, NEG_INF), axis=-1)
    return jnp.where(mask, p, 0.0)


def swiglu(h, wg, wu, wd):
    return (jax.nn.silu(h @ wg) * (h @ wu)) @ wd


def nsa_mixer(h, w_in, gate_b, pe_k, pe_v, ck_w1, ck_w2, cv_w1, cv_w2, w_out):
    B, S, D = h.shape
    G, Hg, HD = N_KV_GROUPS, HEADS_PER_GROUP, HEAD_DIM
    nc = S // CMP_STRIDE - 1
    ns = S // SLC_BLOCK
    n_sel = min(N_SLC, ns)
    nb = S // Q_BLOCK
    pos = jnp.arange(S)

    proj = h @ w_in
    D0 = N_HEADS * HD
    cuts = [D0 + i * KV_WIDTH for i in range(7)]
    q, k_c, v_c, k_s, v_s, k_w, v_w, gates = jnp.split(proj, cuts, axis=-1)
    q = rope(q.reshape(B, S, G, Hg, HD), pos) * (HD ** -0.5)
    k_s = rope(k_s.reshape(B, S, G, HD), pos)
    v_s = v_s.reshape(B, S, G, HD)
    k_w = rope(k_w.reshape(B, S, G, HD), pos)
    v_w = v_w.reshape(B, S, G, HD)
    gates = jax.nn.sigmoid(gates + gate_b).reshape(B, S, 3, G, Hg)

    def compress(t, pe, w1, w2):
        ch = t.reshape(B, S // CMP_STRIDE, CMP_STRIDE, G, HD)
        blk = jnp.concatenate([ch[:, :-1], ch[:, 1:]], axis=2) + pe[None, None, :, None, :]
        blk = blk.transpose(0, 1, 3, 2, 4).reshape(B, nc, G, CMP_BLOCK * HD)
        return jax.nn.gelu(blk @ w1) @ w2

    cmp_pos = jnp.arange(nc) * CMP_STRIDE + CMP_BLOCK - 1
    kc = rope(compress(k_c.reshape(B, S, G, HD), pe_k, ck_w1, ck_w2), cmp_pos)
    vc = compress(v_c.reshape(B, S, G, HD), pe_v, cv_w1, cv_w2)

    kb = k_s.reshape(B, ns, SLC_BLOCK, G, HD).transpose(0, 3, 1, 2, 4)
    vb = v_s.reshape(B, ns, SLC_BLOCK, G, HD).transpose(0, 3, 1, 2, 4)
    kw = jnp.pad(k_w, ((0, 0), (WINDOW, 0), (0, 0), (0, 0)))
    vw = jnp.pad(v_w, ((0, 0), (WINDOW, 0), (0, 0), (0, 0)))
    b_ix = jnp.arange(B)[:, None, None, None]
    g_ix = jnp.arange(G)[None, :, None, None]
    slc_w = jnp.array([1.0, 2.0, 2.0, 2.0], jnp.float32)
    blk_id = jnp.arange(ns)

    def block(qb):
        t0 = qb * Q_BLOCK
        qpos = t0 + jnp.arange(Q_BLOCK)
        qq = lax.dynamic_slice_in_dim(q, t0, Q_BLOCK, axis=1)
        gg = lax.dynamic_slice_in_dim(gates, t0, Q_BLOCK, axis=1)

        s_c = jnp.einsum('bqghd,bcgd->bghqc', qq, kc).astype(jnp.float32)
        m_c = cmp_pos[None, :] <= qpos[:, None]
        p_c = masked_softmax(s_c, m_c)
        o_c = jnp.einsum('bghqc,bcgd->bqghd', p_c.astype(vc.dtype), vc)

        imp = jnp.pad(p_c.sum(axis=2), ((0, 0), (0, 0), (0, 0), (1, RATIO)))
        s_blk = (imp[..., :RATIO * ns].reshape(B, G, Q_BLOCK, ns, RATIO) @ slc_w
                 + imp[..., RATIO:RATIO * ns + 1:RATIO])
        cur = qpos // SLC_BLOCK
        valid = blk_id[None, :] * SLC_BLOCK <= qpos[:, None]
        forced = ((blk_id[None, :] == 0) | (blk_id[None, :] == cur[:, None])
                  | (blk_id[None, :] == cur[:, None] - 1))
        s_blk = jnp.where(forced, FORCE, jnp.where(valid, s_blk, NEG_INF))
        _, idx = lax.top_k(s_blk, n_sel)

        kg = kb[b_ix, g_ix, idx]
        vg = vb[b_ix, g_ix, idx]
        s_s = jnp.einsum('bqghd,bgqnkd->bghqnk', qq, kg).astype(jnp.float32)
        key_pos = idx[..., None] * SLC_BLOCK + jnp.arange(SLC_BLOCK)
        m_s = key_pos <= qpos[None, None, :, None, None]
        p_s = masked_softmax(s_s.reshape(B, G, Hg, Q_BLOCK, n_sel * SLC_BLOCK),
                             m_s[:, :, None].reshape(B, G, 1, Q_BLOCK, n_sel * SLC_BLOCK))
        o_s = jnp.einsum('bghqnk,bgqnkd->bqghd',
                         p_s.reshape(B, G, Hg, Q_BLOCK, n_sel, SLC_BLOCK).astype(vg.dtype), vg)

        kk = lax.dynamic_slice_in_dim(kw, t0, Q_BLOCK + WINDOW, axis=1)
        vv = lax.dynamic_slice_in_dim(vw, t0, Q_BLOCK + WINDOW, axis=1)
        kpos = t0 - WINDOW + jnp.arange(Q_BLOCK + WINDOW)
        diff = qpos[:, None] - kpos[None, :]
        m_w = (diff >= 0) & (diff < WINDOW) & (kpos[None, :] >= 0)
        s_w = jnp.einsum('bqghd,bkgd->bghqk', qq, kk).astype(jnp.float32)
        p_w = masked_softmax(s_w, m_w)
        o_w = jnp.einsum('bghqk,bkgd->bqghd', p_w.astype(vv.dtype), vv)

        o = (gg[:, :, 0, :, :, None] * o_c + gg[:, :, 1, :, :, None] * o_s
             + gg[:, :, 2, :, :, None] * o_w)
        return o.reshape(B, Q_BLOCK, N_HEADS * HD)

    out = lax.map(block, jnp.arange(nb))
    out = out.transpose(1, 0, 2, 3).reshape(B, S, N_HEADS * HD)
    return out @ w_out


def pool_mixer(h, w_pool, b_pool, scale):
    B, S, D = h.shape
    hf = h.astype(jnp.float32)
    c0 = jnp.concatenate([jnp.zeros((B, 1, D), jnp.float32), jnp.cumsum(hf, axis=1)], axis=1)
    pos = jnp.arange(S)
    diffs = []
    for g, w in enumerate(POOL_WINDOWS):
        lo, hi = g * POOL_GROUP, (g + 1) * POOL_GROUP
        cg = c0[:, :, lo:hi]
        lag = jnp.concatenate([jnp.zeros((B, w - 1, POOL_GROUP), jnp.float32), cg[:, :S + 1 - w]], axis=1)
        cnt = jnp.minimum(pos + 1, w).astype(jnp.float32)[None, :, None]
        diffs.append((cg[:, 1:] - lag) / cnt - hf[:, :, lo:hi])
    d = jnp.stack(diffs, axis=2).astype(h.dtype)
    z = jnp.einsum('bsgc,gce->bsge', d, w_pool) + b_pool
    return z.reshape(B, S, D) * scale


def mem_attn(h, memn, wq, wk, wv, wo):
    B, S, _ = h.shape
    M = memn.shape[1]
    q = (h @ wq).reshape(B, S, MEM_HEADS, MEM_HEAD_DIM) * (MEM_HEAD_DIM ** -0.5)
    k = (memn @ wk).reshape(B, M, MEM_HEADS, MEM_HEAD_DIM)
    v = (memn @ wv).reshape(B, M, MEM_HEADS, MEM_HEAD_DIM)
    s = jnp.einsum('bshd,bmhd->bhsm', q, k).astype(jnp.float32)
    p = jax.nn.softmax(s, axis=-1).astype(v.dtype)
    o = jnp.einsum('bhsm,bmhd->bshd', p, v).reshape(B, S, MEM_WIDTH)
    return o @ wo


def moe(h, router, wg, wu, wd):
    B, S, D = h.shape
    N = B * S
    xf = h.reshape(N, D)
    logits = (xf @ router).astype(jnp.float32)
    top_val, top_idx = lax.top_k(logits, TOP_K)
    gate = jax.nn.softmax(top_val, axis=-1)
    e_flat = top_idx.reshape(-1)
    t_flat = jnp.repeat(jnp.arange(N, dtype=jnp.int32), TOP_K)
    w_flat = gate.reshape(-1)
    order = jnp.argsort(e_flat)
    e_sorted, t_sorted, w_sorted = e_flat[order], t_flat[order], w_flat[order]
    counts = jnp.zeros((N_EXPERTS,), jnp.int32).at[e_flat].add(1)
    padded = (counts + MOE_BLOCK - 1) // MOE_BLOCK * MOE_BLOCK
    off = jnp.cumsum(counts) - counts
    poff = jnp.cumsum(padded) - padded
    pend = poff + padded
    dest = poff[e_sorted] + jnp.arange(N * TOP_K, dtype=jnp.int32) - off[e_sorted]
    n_blk = (N * TOP_K + MOE_BLOCK - 1) // MOE_BLOCK + N_EXPERTS
    cap = n_blk * MOE_BLOCK
    tok_buf = jnp.full((cap,), N, jnp.int32).at[dest].set(t_sorted)
    w_buf = jnp.zeros((cap,), jnp.float32).at[dest].set(w_sorted)
    blk_start = jnp.arange(n_blk) * MOE_BLOCK
    blk_exp = jnp.minimum(jnp.sum(pend[None, :] <= blk_start[:, None], axis=1), N_EXPERTS - 1)
    x_pad = jnp.concatenate([xf, jnp.zeros((1, D), xf.dtype)], axis=0)

    def expert_block(args):
        toks, e = args
        return swiglu(x_pad[toks], wg[e], wu[e], wd[e])

    out = lax.map(expert_block, (tok_buf.reshape(n_blk, MOE_BLOCK), blk_exp))
    out = out.reshape(cap, D) * w_buf[:, None].astype(out.dtype)
    y = jnp.zeros((N + 1, D), out.dtype).at[tok_buf].add(out)[:N]
    return y.reshape(B, S, D)


def setup_inputs(seed: int = 0) -> dict:
    key = jax.random.key(seed)
    ks = iter(jax.random.split(key, 32))
    f32 = jnp.float32
    na, nb = N_MIXER_A_LAYERS, N_MIXER_B_LAYERS

    def w(shape, fan_in):
        return jax.random.normal(next(ks), shape, f32) * (fan_in ** -0.5)

    def gain(shape):
        return 1.0 + 0.02 * jax.random.normal(next(ks), shape, f32)

    def small(shape, s):
        return s * jax.random.normal(next(ks), shape, f32)

    return {
        "x": jax.random.normal(next(ks), (BATCH, SEQ, D_MODEL), f32),
        "mem": jax.random.normal(next(ks), (BATCH, MEM_LEN, D_MODEL), f32),
        "norm_mix": gain((DEPTH, D_MODEL)),
        "norm_mem_q": gain((DEPTH, D_MODEL)),
        "norm_mem_kv": gain((DEPTH, D_MODEL)),
        "norm_ffn": gain((DEPTH, D_MODEL)),
        "norm_final": gain((D_MODEL,)),
        "nsa_w_in": w((na, D_MODEL, NSA_IN_WIDTH), D_MODEL),
        "nsa_gate_b": small((na, 3 * N_HEADS), 0.1),
        "nsa_pe_k": small((na, CMP_BLOCK, HEAD_DIM), 0.1),
        "nsa_pe_v": small((na, CMP_BLOCK, HEAD_DIM), 0.1),
        "nsa_cmp_k_w1": w((na, CMP_BLOCK * HEAD_DIM, CMP_HIDDEN), CMP_BLOCK * HEAD_DIM),
        "nsa_cmp_k_w2": w((na, CMP_HIDDEN, HEAD_DIM), CMP_HIDDEN),
        "nsa_cmp_v_w1": w((na, CMP_BLOCK * HEAD_DIM, CMP_HIDDEN), CMP_BLOCK * HEAD_DIM),
        "nsa_cmp_v_w2": w((na, CMP_HIDDEN, HEAD_DIM), CMP_HIDDEN),
        "nsa_w_out": w((na, N_HEADS * HEAD_DIM, D_MODEL), N_HEADS * HEAD_DIM),
        "pool_w": w((nb, N_POOL_GROUPS, POOL_GROUP, POOL_GROUP), POOL_GROUP),
        "pool_b": small((nb, N_POOL_GROUPS, POOL_GROUP), 0.01),
        "pool_scale": gain((nb, D_MODEL)),
        "mem_wq": w((DEPTH, D_MODEL, MEM_WIDTH), D_MODEL),
        "mem_wk": w((DEPTH, D_MODEL, MEM_WIDTH), D_MODEL),
        "mem_wv": w((DEPTH, D_MODEL, MEM_WIDTH), D_MODEL),
        "mem_wo": w((DEPTH, MEM_WIDTH, D_MODEL), MEM_WIDTH),
        "ffn_w_gate": w((na, D_MODEL, D_FF), D_MODEL),
        "ffn_w_up": w((na, D_MODEL, D_FF), D_MODEL),
        "ffn_w_down": w((na, D_FF, D_MODEL), D_FF),
        "moe_router": w((nb, D_MODEL, N_EXPERTS), D_MODEL),
        "moe_w_gate": w((nb, N_EXPERTS, D_MODEL, D_FF), D_MODEL),
        "moe_w_up": w((nb, N_EXPERTS, D_MODEL, D_FF), D_MODEL),
        "moe_w_down": w((nb, N_EXPERTS, D_FF, D_MODEL), D_FF),
    }


def reference(x, mem, norm_mix, norm_mem_q, norm_mem_kv, norm_ffn, norm_final,
              nsa_w_in, nsa_gate_b, nsa_pe_k, nsa_pe_v, nsa_cmp_k_w1, nsa_cmp_k_w2,
              nsa_cmp_v_w1, nsa_cmp_v_w2, nsa_w_out,
              pool_w, pool_b, pool_scale,
              mem_wq, mem_wk, mem_wv, mem_wo,
              ffn_w_gate, ffn_w_up, ffn_w_down,
              moe_router, moe_w_gate, moe_w_up, moe_w_down):
    h = x
    for i in range(DEPTH):
        j = i // 2
        u = rmsnorm(h, norm_mix[i])
        if i % 2 == 0:
            h = h + nsa_mixer(u, nsa_w_in[j], nsa_gate_b[j], nsa_pe_k[j], nsa_pe_v[j],
                              nsa_cmp_k_w1[j], nsa_cmp_k_w2[j], nsa_cmp_v_w1[j], nsa_cmp_v_w2[j],
                              nsa_w_out[j])
        else:
            h = h + pool_mixer(u, pool_w[j], pool_b[j], pool_scale[j])
        memn = rmsnorm(mem, norm_mem_kv[i])
        h = h + mem_attn(rmsnorm(h, norm_mem_q[i]), memn, mem_wq[i], mem_wk[i], mem_wv[i], mem_wo[i])
        u = rmsnorm(h, norm_ffn[i])
        if i % 2 == 0:
            h = h + swiglu(u, ffn_w_gate[j], ffn_w_up[j], ffn_w_down[j])
        else:
            h = h + moe(u, moe_router[j], moe_w_gate[j], moe_w_up[j], moe_w_down[j])
    return rmsnorm(h, norm_final)
```

```python
import numpy as np
import concourse.bass as bass
import concourse.mybir as mybir
from concourse.bass_utils import run_bass_kernel_spmd

F32 = mybir.dt.float32
BF16 = mybir.dt.bfloat16
AF = mybir.ActivationFunctionType
ALU = mybir.AluOpType
AX = mybir.AxisListType


class Res:
    __slots__ = ("lw", "rd")

    def __init__(self):
        self.lw = None
        self.rd = {}


class Sched:
    ENG = ("pe", "act", "dve", "pool", "sp")
    NDMA = 6

    def __init__(self, nc, stack):
        self.nc = nc
        self.prog = {e: [] for e in self.ENG}
        self.sems = {}
        for e in ("pe", "act", "dve", "pool"):
            self.sems[e] = stack.enter_context(nc.semaphore("s_" + e))
        self.cnt = {e: 0 for e in self.ENG}
        self.known = {e: {} for e in self.ENG}
        self.dq = {}
        for q in ("sp", "pool", "act"):
            sl = []
            for i in range(self.NDMA):
                k = "d_%s%d" % (q, i)
                self.sems[k] = stack.enter_context(nc.semaphore(k))
                sl.append(k)
            self.dq[q] = [sl, 0]
        self.nwait = 0
        self.off = False
        self.ncc = 0
        self.sems["s_cc"] = stack.enter_context(nc.semaphore("s_cc"))

    def _wait(self, eng, key, val):
        if val <= 0:
            return
        kn = self.known[eng]
        if kn.get(key, 0) >= val:
            return
        kn[key] = val
        sem = self.sems[key]
        self.nwait += 1
        self.prog[eng].append(lambda E, sem=sem, val=val: E.wait_ge(sem, val))

    def _deps(self, eng, reads, writes):
        for r in reads:
            if r.lw is not None:
                self._wait(eng, *r.lw)
        for w in writes:
            if w.lw is not None:
                self._wait(eng, *w.lw)
            for k, v in w.rd.items():
                self._wait(eng, k, v)

    def op(self, eng, fn, reads=(), writes=()):
        if self.off:
            return
        self._deps(eng, reads, writes)
        self.cnt[eng] += 1
        idx = self.cnt[eng]
        sem = self.sems[eng]
        self.prog[eng].append(lambda E, fn=fn, sem=sem: fn(E).then_inc(sem, 1))
        if eng == "pe":
            self.known[eng][eng] = idx
        for r in reads:
            r.rd[eng] = idx
        for w in writes:
            w.lw = (eng, idx)
            w.rd = {}

    def dma(self, q, out, in_, reads=(), writes=()):
        if self.off:
            return None
        sl, n = self.dq[q]
        key = sl[n % self.NDMA]
        val = 16 * (n // self.NDMA + 1)
        self.dq[q][1] = n + 1
        self._wait(q, key, val - 16)
        self._deps(q, reads, writes)
        sem = self.sems[key]
        self.prog[q].append(lambda E, out=out, in_=in_, sem=sem: E.dma_start(out=out, in_=in_).then_inc(sem, 16))
        for r in reads:
            r.rd[key] = val
        for w in writes:
            w.lw = (key, val)
            w.rd = {}
        return key, val

    def coll(self, kind, op, ins, outs, reads=(), writes=()):
        if self.off:
            return None
        q = "pool"
        key = "s_cc"
        self.ncc += 1
        val = self.ncc
        self._wait(q, key, val - 1)
        self._deps(q, reads, writes)
        sem = self.sems[key]
        self.prog[q].append(lambda E, sem=sem: E.collective_compute(kind, op, replica_groups=[list(range(8))],
                                                                    ins=[a_.opt() for a_ in ins], outs=[a_.opt() for a_ in outs]).then_inc(sem))
        for r in reads:
            r.rd[key] = val
        for w in writes:
            w.lw = (key, val)
            w.rd = {}
        return key, val

    def barrier(self):
        tg = {e: self.cnt[e] for e in ("pe", "act", "dve", "pool")}
        tg["s_cc"] = self.ncc
        for q, (sl, n) in self.dq.items():
            for i, key in enumerate(sl):
                tg[key] = 16 * ((n - i + self.NDMA - 1) // self.NDMA) if n > i else 0
        for eng in (self.BENG if hasattr(self, 'BENG') else self.ENG):
            for k, v in tg.items():
                self._wait(eng, k, v)

    def finish(self, pending):
        for key, val in pending:
            self._wait("sp", key, val)
        nc = self.nc
        with nc.Block() as block:
            @block.sync
            def _(E):
                for f in self.prog["sp"]:
                    f(E)

            @block.tensor
            def _(E):
                for f in self.prog["pe"]:
                    f(E)

            @block.scalar
            def _(E):
                for f in self.prog["act"]:
                    f(E)

            @block.vector
            def _(E):
                for f in self.prog["dve"]:
                    f(E)

            @block.gpsimd
            def _(E):
                for f in self.prog["pool"]:
                    f(E)


import ml_dtypes
from contextlib import ExitStack

NPBF = ml_dtypes.bfloat16
NCORES = 8
D = 2048
SEQ = 16384
TPC = SEQ // NCORES
EPS = 1e-6


class Ctx:
    def __init__(self):
        self.nc = bass.Bass("TRN2", target_bir_lowering=False)
        self.st = ExitStack()
        self.S = Sched(self.nc, self.st)
        self.n = 0
        self.out_pending = []

    def din(self, name, shape, dt):
        return self.nc.dram_tensor(name, list(shape), dt, kind="ExternalInput").ap()

    def dout(self, name, shape, dt):
        return self.nc.dram_tensor(name, list(shape), dt, kind="ExternalOutput").ap()

    def sb(self, name, shape, dt):
        t = self.st.enter_context(self.nc.sbuf_tensor(name, list(shape), dt))
        return t, Res()

    def ps(self, name, shape, dt=F32):
        t = self.st.enter_context(self.nc.psum_tensor(name, list(shape), dt))
        return t, Res()

    def load(self, q, out, in_, w):
        self.S.dma(q, out, in_, writes=[w])

    def store(self, q, out, in_, r):
        kv = self.S.dma(q, out, in_, reads=[r])
        if kv is not None:
            self.out_pending.append(kv)

    def finish(self):
        self.S.finish(self.out_pending)
        self.st.close()
        return self.nc


def rmsnorm_tile(c, xt, rx, gt, rg, u, ru, junk, rj, ss, rss):
    S = c.S
    S.op("act", lambda E: E.activation(out=junk[:], in_=xt[:], func=AF.Square, accum_out=ss[:, 0:1]),
         reads=[rx], writes=[rj, rss])
    S.op("dve", lambda E: E.tensor_scalar(out=ss[:, 1:2], in0=ss[:, 0:1], scalar1=1.0 / D, scalar2=EPS,
                                          op0=ALU.mult, op1=ALU.add), reads=[rss], writes=[rss])
    S.op("act", lambda E: E.activation(out=ss[:, 1:2], in_=ss[:, 1:2], func=AF.Sqrt), reads=[rss], writes=[rss])
    S.op("dve", lambda E: E.reciprocal(out=ss[:, 1:2], in_=ss[:, 1:2]), reads=[rss], writes=[rss])
    S.op("dve", lambda E: E.scalar_tensor_tensor(out=u[:], in0=xt[:], scalar=ss[:, 1:2], in1=gt[:],
                                                 op0=ALU.mult, op1=ALU.mult), reads=[rx, rss, rg], writes=[ru])


def transpose_to(c, src, rsrc, nchunk, dst_fn, rdst, pT, rpT, idt, rid, eng_cycle=("dve", "act")):
    S = c.S
    for k0 in range(0, nchunk, 4):
        n = min(4, nchunk - k0)
        for k in range(n):
            S.op("pe", lambda E, k=k, k0=k0: E.transpose(out=pT[:, k, :], in_=src[:, (k0 + k) * 128:(k0 + k + 1) * 128],
                                                      identity=idt[:]), reads=[rsrc, rid], writes=[rpT])
        eng = eng_cycle[(k0 // 4) % len(eng_cycle)]
        if eng == "act":
            S.op("act", lambda E, k0=k0, n=n: E.activation(out=dst_fn(k0, n), in_=pT[:, 0:n, :], func=AF.Copy),
                 reads=[rpT], writes=[rdst])
        else:
            S.op(eng, lambda E, k0=k0, n=n: E.tensor_copy(out=dst_fn(k0, n), in_=pT[:, 0:n, :]),
                 reads=[rpT], writes=[rdst])


NSA_W = 5168


def build_k1(ntile=16):
    c = Ctx()
    S = c.S
    T = ntile * 128
    x = c.din("x", [T, D], F32)
    g = c.din("g", [D], F32)
    w = c.din("w", [D, NSA_W], F32)
    gb = c.din("gb", [48], F32)
    ropet = c.din("ropet", [4, T, 64], F32)
    ident = c.din("ident", [128, 128], BF16)
    fT = c.dout("fT", [8, 4, 128, T], BF16)
    vtok = c.dout("vtok", [2, T, 512], BF16)
    gates = c.dout("gates", [T, 48], F32)

    uT, ruT = c.sb("uT", [128, 16, T], BF16)
    gt, rgt = c.sb("gt", [128, D], F32)
    idt, rid = c.sb("idt", [128, 128], BF16)
    gbt, rgb = c.sb("gbt", [128, 48], F32)
    rt, rrt = c.sb("rt", [128, 4, ntile, 64], F32)
    xts = [c.sb("xt%d" % i, [128, D], F32) for i in range(2)]
    junk, rj = c.sb("junk", [128, D], BF16)
    ss, rss = c.sb("ss", [128, 2], F32)
    u, ru = c.sb("u", [128, D], BF16)
    wts = [c.sb("wt%d" % i, [128, 16, 512], BF16) for i in range(2)]
    xss = [c.sb("xs%d" % i, [128, 4, 128], F32) for i in range(2)]
    tmp = [c.sb("tmp%d" % i, [128, 4, 64], F32) for i in range(4)]
    rbs = [c.sb("rb%d" % i, [128, 4, 128], BF16) for i in range(2)]
    tss = [c.sb("ts%d" % i, [128, 4, 128], BF16) for i in range(2)]
    gss = [c.sb("gs%d" % i, [128, 48], F32) for i in range(2)]
    pT, rpT = c.ps("pT", [128, 4, 128], BF16)
    pys = [c.ps("py%d" % i, [128, 512], F32) for i in range(2)]

    c.load("sp", gt[:], g.partition_broadcast(128), rgt)
    c.load("sp", idt[:], ident, rid)
    c.load("sp", gbt[:], gb.partition_broadcast(128), rgb)
    for i in range(4):
        c.load("sp", rt[:, i, :, :], ropet[i].rearrange("(t p) d -> p t d", p=128), rrt)

    for tt in range(ntile):
        xt, rx = xts[tt % 2]
        c.load("sp", xt[:], x[tt * 128:(tt + 1) * 128, :], rx)
        rmsnorm_tile(c, xt, rx, gt, rgt, u, ru, junk, rj, ss, rss)
        transpose_to(c, u, ru, 16, lambda k0, n, tt=tt: uT[:, k0:k0 + n, tt * 128:(tt + 1) * 128], ruT, pT, rpT, idt, rid)

    nblk = 11
    it = 0
    for cb in range(nblk):
        ncol = min(512, NSA_W - cb * 512)
        wt, rw = wts[cb % 2]
        c.load("pool", wt[:, :, 0:ncol], w[:, cb * 512:cb * 512 + ncol].rearrange("(kc p) n -> p kc n", p=128), rw)
        for tt in range(ntile):
            py, rpy = pys[it % 2]
            xs, rxs = xss[it % 2]
            rb, rrb = rbs[it % 2]
            ts_, rts = tss[it % 2]
            gs, rgs = gss[it % 2]
            it += 1
            for kc in range(16):
                S.op("pe", lambda E, kc=kc, py=py, wt=wt, tt=tt, ncol=ncol: E.matmul(
                    py[:, 0:ncol], lhsT=uT[:, kc, tt * 128:(tt + 1) * 128], rhs=wt[:, kc, 0:ncol],
                    start=(kc == 0), stop=(kc == 15)), reads=[ruT, rw], writes=[rpy])
            pyv = py[:].rearrange("p (h d) -> p h d", h=4)
            if cb in (0, 1, 2, 3, 6, 8):
                ci = 0 if cb < 4 else 2
                cos = rt[:, ci, tt, :].unsqueeze(1).to_broadcast([128, 4, 64])
                sin = rt[:, ci + 1, tt, :].unsqueeze(1).to_broadcast([128, 4, 64])
                S.op("act", lambda E, xs=xs, pyv=pyv: E.activation(out=xs[:], in_=pyv, func=AF.Copy), reads=[rpy], writes=[rxs])
                x1 = xs[:, :, 0:64]
                x2 = xs[:, :, 64:128]
                (t1, r1), (t2, r2), (t3, r3), (t4, r4) = tmp
                S.op("dve", lambda E, x1=x1, cos=cos, t1=t1: E.tensor_tensor(out=t1[:], in0=x1, in1=cos, op=ALU.mult), reads=[rxs, rrt], writes=[r1])
                S.op("dve", lambda E, x2=x2, sin=sin, t2=t2: E.tensor_tensor(out=t2[:], in0=x2, in1=sin, op=ALU.mult), reads=[rxs, rrt], writes=[r2])
                S.op("dve", lambda E, rb=rb, t1=t1, t2=t2: E.tensor_tensor(out=rb[:, :, 0:64], in0=t1[:], in1=t2[:], op=ALU.subtract), reads=[r1, r2], writes=[rrb])
                S.op("dve", lambda E, x2=x2, cos=cos, t3=t3: E.tensor_tensor(out=t3[:], in0=x2, in1=cos, op=ALU.mult), reads=[rxs, rrt], writes=[r3])
                S.op("dve", lambda E, x1=x1, sin=sin, t4=t4: E.tensor_tensor(out=t4[:], in0=x1, in1=sin, op=ALU.mult), reads=[rxs, rrt], writes=[r4])
                S.op("dve", lambda E, rb=rb, t3=t3, t4=t4: E.tensor_tensor(out=rb[:, :, 64:128], in0=t3[:], in1=t4[:], op=ALU.add), reads=[r3, r4], writes=[rrb])
            elif cb == 10:
                S.op("dve", lambda E, gs=gs, py=py: E.tensor_tensor(out=gs[:], in0=py[:, 0:48], in1=gbt[:], op=ALU.add), reads=[rpy, rgb], writes=[rgs])
                S.op("act", lambda E, gs=gs: E.activation(out=gs[:], in_=gs[:], func=AF.Sigmoid), reads=[rgs], writes=[rgs])
                c.store("sp", gates[tt * 128:(tt + 1) * 128, :], gs[:], rgs)
                continue
            else:
                S.op("act", lambda E, rb=rb, pyv=pyv: E.activation(out=rb[:], in_=pyv, func=AF.Copy), reads=[rpy], writes=[rrb])
            if cb in (7, 9):
                c.store("sp", vtok[0 if cb == 7 else 1, tt * 128:(tt + 1) * 128, :], rb[:].rearrange("p h d -> p (h d)"), rrb)
            else:
                bi = {0: 0, 1: 1, 2: 2, 3: 3, 4: 4, 5: 5, 6: 6, 8: 7}[cb]
                rbf = rb[:].rearrange("p h d -> p (h d)")
                transpose_to(c, rbf, rrb, 4, lambda k0, n, ts_=ts_: ts_[:, k0:k0 + n, :], rts, pT, rpT, idt, rid)
                c.store("sp", fT[bi, :, :, tt * 128:(tt + 1) * 128].rearrange("h d t -> d h t"), ts_[:], rts)
    return c.finish()


def rope_tables(pos, scale=1.0):
    half = 64
    inv = (10000.0 ** (-np.arange(half, dtype=np.float32) / half)).astype(np.float32)
    ang = pos.astype(np.float32)[:, None] * inv[None, :]
    return (np.cos(ang) * scale).astype(np.float32), (np.sin(ang) * scale).astype(np.float32)


def build_k2():
    c = Ctx()
    S = c.S
    XT = 2048 + 16
    xT_d = c.din("xT", [2, 4, 128, XT], BF16)
    w1_d = c.din("w1", [2, 4096, 256], F32)
    w2_d = c.din("w2", [2, 256, 128], F32)
    peT_d = c.din("peT", [2, 128, 32], F32)
    ropec = c.din("ropec", [2, 128, 64], F32)
    ident = c.din("ident", [128, 128], BF16)
    kcT_o = c.dout("kcT", [4, 128, 128], BF16)
    vc_o = c.dout("vc", [128, 4, 128], BF16)

    xT, rxT = c.sb("xTs", [128, 2, 4, XT], BF16)
    w1, rw1 = c.sb("w1s", [128, 2, 32, 256], BF16)
    w2, rw2 = c.sb("w2s", [128, 2, 2, 128], BF16)
    pef, rpef = c.sb("pef", [128, 2, 32], F32)
    peb, rpeb = c.sb("peb", [128, 2, 32], BF16)
    rc, rrc = c.sb("rc", [128, 2, 64], F32)
    idt, rid = c.sb("idt", [128, 128], BF16)
    bias, rbias = c.sb("bias", [128, 4], F32)
    xa, rxa = c.sb("xa", [128, 128], F32)
    t1, r1 = c.sb("t1", [128, 128], F32)
    t2, r2 = c.sb("t2", [128, 128], F32)
    h1T, rh1 = c.sb("h1T", [128, 2, 128], BF16)
    xs, rxs = c.sb("xs", [128, 128], F32)
    rb, rrb = c.sb("rb", [128, 128], BF16)
    kst, rkst = c.sb("kst", [128, 4, 128], BF16)
    vst, rvst = c.sb("vst", [128, 4, 128], BF16)
    pb, rpb = c.ps("pb", [128, 4], F32)
    ph, rph = c.ps("ph", [128, 128], F32)
    pk, rpk = c.ps("pk", [128, 128], F32)
    pT, rpT = c.ps("pT", [128, 4, 128], BF16)

    c.load("sp", xT[:], xT_d.rearrange("a g d t -> d a g t"), rxT)
    for kv in range(2):
        c.load("pool", w1[:, kv, :, :], w1_d[kv].rearrange("(p d) j -> d p j", d=128), rw1)
        c.load("pool", w2[:, kv, :, :], w2_d[kv].rearrange("(jh j) d -> j jh d", j=128), rw2)
    c.load("sp", pef[:], peT_d.rearrange("a d p -> d a p"), rpef)
    c.load("sp", rc[:], ropec.rearrange("a c d -> c a d"), rrc)
    c.load("sp", idt[:], ident, rid)
    S.op("dve", lambda E: E.tensor_copy(out=peb[:], in_=pef[:]), reads=[rpef], writes=[rpeb])
    for kv in range(2):
        for jh in range(2):
            for p in range(32):
                S.op("pe", lambda E, kv=kv, jh=jh, p=p: E.matmul(
                    pb[:, kv * 2 + jh:kv * 2 + jh + 1], lhsT=w1[:, kv, p, jh * 128:(jh + 1) * 128], rhs=peb[:, kv, p:p + 1],
                    start=(p == 0), stop=(p == 31)), reads=[rw1, rpeb], writes=[rpb])
    S.op("dve", lambda E: E.tensor_copy(out=bias[:], in_=pb[:]), reads=[rpb], writes=[rbias])
    for kv in range(2):
        for g in range(4):
            for jh in range(2):
                for p in range(32):
                    S.op("pe", lambda E, kv=kv, jh=jh, p=p, g=g: E.matmul(
                        ph[:], lhsT=w1[:, kv, p, jh * 128:(jh + 1) * 128], rhs=xT[:, kv, g, p:p + 2033:16],
                        start=(p == 0), stop=(p == 31)), reads=[rw1, rxT], writes=[rph])
                S.op("act", lambda E, kv=kv, jh=jh: E.activation(out=xa[:], in_=ph[:], func=AF.Identity,
                                                                 bias=bias[:, kv * 2 + jh:kv * 2 + jh + 1]), reads=[rph, rbias], writes=[rxa])
                S.op("dve", lambda E: E.tensor_tensor(out=t1[:], in0=xa[:], in1=xa[:], op=ALU.mult), reads=[rxa], writes=[r1])
                S.op("dve", lambda E: E.tensor_scalar(out=t1[:], in0=t1[:], scalar1=0.044715, scalar2=1.0, op0=ALU.mult, op1=ALU.add), reads=[r1], writes=[r1])
                S.op("dve", lambda E: E.tensor_tensor(out=t1[:], in0=t1[:], in1=xa[:], op=ALU.mult), reads=[r1, rxa], writes=[r1])
                S.op("act", lambda E: E.activation(out=t2[:], in_=t1[:], func=AF.Sigmoid, scale=1.5957691216057308), reads=[r1], writes=[r2])
                S.op("dve", lambda E, jh=jh: E.tensor_tensor(out=h1T[:, jh, :], in0=xa[:], in1=t2[:], op=ALU.mult), reads=[rxa, r2], writes=[rh1])
            for jh in range(2):
                S.op("pe", lambda E, kv=kv, jh=jh: E.matmul(pk[:], lhsT=h1T[:, jh, :], rhs=w2[:, kv, jh, :], start=(jh == 0), stop=(jh == 1)),
                     reads=[rh1, rw2], writes=[rpk])
            if kv == 0:
                S.op("act", lambda E: E.activation(out=xs[:], in_=pk[:], func=AF.Copy), reads=[rpk], writes=[rxs])
                cos = rc[:, 0, :]
                sin = rc[:, 1, :]
                S.op("dve", lambda E: E.tensor_tensor(out=t1[:, 0:64], in0=xs[:, 0:64], in1=cos, op=ALU.mult), reads=[rxs, rrc], writes=[r1])
                S.op("dve", lambda E: E.tensor_tensor(out=t1[:, 64:128], in0=xs[:, 64:128], in1=sin, op=ALU.mult), reads=[rxs, rrc], writes=[r1])
                S.op("dve", lambda E: E.tensor_tensor(out=rb[:, 0:64], in0=t1[:, 0:64], in1=t1[:, 64:128], op=ALU.subtract), reads=[r1], writes=[rrb])
                S.op("dve", lambda E: E.tensor_tensor(out=t2[:, 0:64], in0=xs[:, 64:128], in1=cos, op=ALU.mult), reads=[rxs, rrc], writes=[r2])
                S.op("dve", lambda E: E.tensor_tensor(out=t2[:, 64:128], in0=xs[:, 0:64], in1=sin, op=ALU.mult), reads=[rxs, rrc], writes=[r2])
                S.op("dve", lambda E: E.tensor_tensor(out=rb[:, 64:128], in0=t2[:, 0:64], in1=t2[:, 64:128], op=ALU.add), reads=[r2], writes=[rrb])
                S.op("pe", lambda E: E.transpose(out=pT[:, 0, :], in_=rb[:], identity=idt[:]), reads=[rrb, rid], writes=[rpT])
                S.op("dve", lambda E, g=g: E.tensor_copy(out=kst[:, g, :], in_=pT[:, 0, :]), reads=[rpT], writes=[rkst])
            else:
                S.op("act", lambda E, g=g: E.activation(out=vst[:, g, :], in_=pk[:], func=AF.Copy), reads=[rpk], writes=[rvst])
    c.store("sp", kcT_o.rearrange("g d c -> d g c"), kst[:], rkst)
    c.store("sp", vc_o, vst[:], rvst)
    return c.finish()


POOLENG = 'dve'


def build_k3(nround=16, stage=99):
    c = Ctx()

    def chk(n):
        if stage == n:
            c.S.off = True
    S = c.S
    NKT = 8 * nround
    NCK = (nround - 1) // 2 + 1
    qT_d = c.din("qT", [nround, 16, 128, 128], BF16)
    ksT_d = c.din("ksT", [4, 128, SEQ], BF16)
    vs_d = c.din("vs", [SEQ, 4, 128], BF16)
    kcT_d = c.din("kcT", [4, 128, 1024], BF16)
    vc_d = c.din("vc", [1024, 4, 128], BF16)
    kwT_d = c.din("kwT", [nround, 4, 128, 640], BF16)
    vw_d = c.din("vw", [nround, 640, 4, 128], BF16)
    gates_d = c.din("gates", [nround, 128, 48], F32)
    cmask_d = c.din("cmask", [nround, 2, 128, 128], BF16)
    dmask_d = c.din("dmask", [8, 128, 128], BF16)
    wmask_d = c.din("wmask", [nround, 5, 128, 128], BF16)
    fadd_d = c.din("fadd", [nround, 128, 256], F32)
    E_d = c.din("Econst", [64, 128, 128], BF16)
    W_d = c.din("Wones", [8, 128, 257], BF16)
    ident = c.din("ident", [128, 128], BF16)
    o_d = c.dout("o", [nround, 128, 2048], BF16)

    ks_sb, _rks0 = c.sb("ks_sb", [128, NKT * 128], BF16)
    vs_sb, _rvs0 = c.sb("vs_sb", [128, NKT, 129], BF16)
    NCH = (NKT + 31) // 32
    rks = [Res() for _ in range(NCH)]
    rvs = [Res() for _ in range(NCH)]
    kc_sb, rkc = c.sb("kc_sb", [128, 4, 1024], BF16)
    R_sb, rR = c.sb("R_sb", [128, 4, 8, 385], BF16)
    E_sb, rE = c.sb("E_sb", [128, 64, 128], BF16)
    idt, rid = c.sb("idt", [128, 128], BF16)
    dm_sb, rdm = c.sb("dm_sb", [128, 8, 128], BF16)
    q_sbs = [c.sb("q_sb%d" % i, [128, 4, 128], BF16) for i in range(2)]
    pTs = [c.sb("pTs%d" % i, [128, 4, 128], BF16) for i in range(3)]
    kw_sbs = [c.sb("kw_sb%d" % i, [128, 640], BF16) for i in range(2)]
    vw_sbs = [c.sb("vw_sb%d" % i, [128, 5, 129], BF16) for i in range(2)]
    cm_sbs = [c.sb("cm_sb%d" % i, [128, 2, 128], BF16) for i in range(2)]
    wm_sbs = [c.sb("wm_sb%d" % i, [128, 5, 128], BF16) for i in range(2)]
    fa_sbs = [c.sb("fa_sb%d" % i, [128, 256], F32) for i in range(2)]
    gt_sbs = [c.sb("gt_sb%d" % i, [128, 48], F32) for i in range(2)]
    cs, rcs = c.sb("cs", [128, 4, 385], F32)
    os_, ros = c.sb("os", [128, 4, 129], F32)
    ow, row = c.sb("ow", [128, 4, 129], F32)
    den, rden = c.sb("den", [128, 3, 4], F32)
    coef, rcoef = c.sb("coef", [128, 3, 4], F32)
    sblk, rsblk = c.sb("sblk", [128, 256], F32)
    wk, rwk = c.sb("wk", [128, 256], F32)
    m8, rm8 = c.sb("m8", [128, 8], F32)
    thr, rthr = c.sb("thr", [128, 1], F32)
    sel, rsel = c.sb("sel", [128, 256], BF16)
    selT, rselT = c.sb("selT", [128, 2, 128], BF16)
    selT4, rselT4 = c.sb("selT4", [128, 2, 4, 128], BF16)
    oacc, roacc = c.sb("oacc", [128, 4, 128], F32)
    otmp, rotmp = c.sb("otmp", [128, 4, 128], F32)
    o_sbs = [c.sb("o_sb%d" % i, [128, 4, 128], BF16) for i in range(2)]

    SB = [c.ps("SB%d" % i, [128, 4, 128], F32) for i in range(2)]
    MB, _ = c.ps("MB", [128, 4, 128], F32)
    rMB = [Res() for _ in range(4)]
    TB, rTB = c.ps("TB", [128, 8, 128], BF16)
    ACC = [c.ps("ACC%d" % i, [128, 512], F32) for i in range(4)]

    c.load("sp", kc_sb[:], kcT_d.rearrange("g d c -> d g c"), rkc)
    for g in range(4):
        c.load("sp", R_sb[:, g, :, 0:128], vc_d[:, g, :].rearrange("(ch p) d -> p ch d", p=128), rR)
        c.load("sp", R_sb[:, g, :, 128:385], W_d.rearrange("ch p w -> p ch w"), rR)
    c.load("sp", E_sb[:], E_d.rearrange("m b k -> b m k"), rE)
    c.load("sp", idt[:], ident, rid)
    c.load("sp", dm_sb[:], dmask_d.rearrange("j k q -> k j q"), rdm)
    S.op("dve", lambda E: E.memset(vs_sb[:, :, 128:129], 1.0), writes=rvs)
    for i in range(2):
        S.op("dve", lambda E, i=i: E.memset(vw_sbs[i][0][:, :, 128:129], 1.0), writes=[vw_sbs[i][1]])

    state = {"s": 0, "p": 0, "m": 0}

    def branch(n, q_sb, rq, k_fn, rk, v_fn, rv, ncol, pre_fn, post_fn):
        def issue_s(kt):
            sb, rsb = SB[state["s"] % 2]
            state["s"] += 1
            S.op("pe", lambda E, kt=kt, sb=sb: E.matmul(sb[:].rearrange("p h q -> p (h q)"), lhsT=k_fn(kt),
                                                       rhs=q_sb[:].rearrange("p h q -> p (h q)"), start=True, stop=(pre_fn is None)),
                 reads=[rk(kt) if callable(rk) else rk, rq], writes=[rsb])
            if pre_fn:
                pre_fn(kt, sb, rsb)
            return sb, rsb, None
        nxt = issue_s(0)
        for kt in range(n):
            sb, rsb, extra = nxt
            if kt + 1 < n:
                nxt = issue_s(kt + 1)
            pT, rpT = pTs[state["p"] % 3]
            state["p"] += 1
            S.op("act", lambda E, pT=pT, sb=sb: E.activation(out=pT[:], in_=sb[:], func=AF.Exp), reads=[rsb], writes=[rpT])
            post_fn(kt, pT, rpT, extra)
            for h in range(4):
                acc, racc = ACC[h]
                S.op("pe", lambda E, h=h, kt=kt, pT=pT, acc=acc: E.matmul(acc[:, 0:ncol], lhsT=pT[:, h, :], rhs=v_fn(kt),
                                                                        start=(kt == 0), stop=(kt == n - 1)),
                     reads=[rpT, rv(kt) if callable(rv) else rv], writes=[racc])

    def bc(ap2d):
        return ap2d.unsqueeze(1).to_broadcast([128, 4, 128])

    chk(0)
    it = 0
    for g in range(4):
        for t0 in range(0, NKT, 32):
            t1 = min(NKT, t0 + 32)
            c.load("sp", ks_sb[:, t0 * 128:t1 * 128], ksT_d[g, :, t0 * 128:t1 * 128], rks[t0 // 32])
            c.load("sp", vs_sb[:, t0:t1, 0:128], vs_d[t0 * 128:t1 * 128, g, :].rearrange("(t p) d -> p t d", p=128), rvs[t0 // 32])
        for r in range(nround):
            b = it % 2
            it += 1
            q_sb, rq = q_sbs[b]
            kw_sb, rkw = kw_sbs[b]
            vw_sb, rvw = vw_sbs[b]
            cm_sb, rcm = cm_sbs[b]
            wm_sb, rwm = wm_sbs[b]
            fa_sb, rfa = fa_sbs[b]
            gt_sb, rgt = gt_sbs[b]
            o_sb, ro = o_sbs[b]
            c.load("sp", q_sb[:], qT_d[r, 4 * g:4 * g + 4].rearrange("h d q -> d h q"), rq)
            c.load("sp", cm_sb[:], cmask_d[r].rearrange("a k q -> k a q"), rcm)
            c.load("sp", fa_sb[:], fadd_d[r], rfa)
            c.load("sp", gt_sb[:], gates_d[r], rgt)
            c.load("sp", kw_sb[:], kwT_d[r, g], rkw)
            c.load("sp", vw_sb[:, :, 0:128], vw_d[r, :, g, :].rearrange("(j p) d -> p j d", p=128), rvw)
            c.load("sp", wm_sb[:], wmask_d[r].rearrange("j k q -> k j q"), rwm)
            nck = r // 2 + 1
            nsc = r // 8 + 1
            nkt = 8 * (r + 1)
            chk(1)

            def cmp_post(kt, pT, rpT, extra, nck=nck, cm_sb=cm_sb, rcm=rcm):
                if kt == nck - 1:
                    S.op("dve", lambda E: E.tensor_tensor(out=pT[:], in0=pT[:], in1=bc(cm_sb[:, 1, :]), op=ALU.mult), reads=[rpT, rcm], writes=[rpT])
                elif kt == nck - 2:
                    S.op("dve", lambda E: E.tensor_tensor(out=pT[:], in0=pT[:], in1=bc(cm_sb[:, 0, :]), op=ALU.mult), reads=[rpT, rcm], writes=[rpT])
            branch(nck, q_sb, rq, lambda kt, g=g: kc_sb[:, g, kt * 128:(kt + 1) * 128], rkc,
                   lambda kt, g=g: R_sb[:, g, kt, :], rR, 385, None, cmp_post)
            for h in range(4):
                acc, racc = ACC[h]
                if h % 2 == 0:
                    S.op("act", lambda E, h=h, acc=acc: E.activation(out=cs[:, h, :], in_=acc[:, 0:385], func=AF.Copy), reads=[racc], writes=[rcs])
                else:
                    S.op("dve", lambda E, h=h, acc=acc: E.tensor_copy(out=cs[:, h, :], in_=acc[:, 0:385]), reads=[racc], writes=[rcs])
            chk(5)
            def win_post(kt, pT, rpT, extra, wm_sb=wm_sb, rwm=rwm):
                S.op(POOLENG, lambda E: E.tensor_tensor(out=pT[:], in0=pT[:], in1=bc(wm_sb[:, kt, :]), op=ALU.mult), reads=[rpT, rwm], writes=[rpT])
            branch(5, q_sb, rq, lambda kt, kw_sb=kw_sb: kw_sb[:, kt * 128:(kt + 1) * 128], rkw,
                   lambda kt, vw_sb=vw_sb: vw_sb[:, kt, :], rvw, 129, None, win_post)
            for h in range(4):
                acc, racc = ACC[h]
                if h % 2 == 0:
                    S.op("act", lambda E, h=h, acc=acc: E.activation(out=ow[:, h, :], in_=acc[:, 0:129], func=AF.Copy), reads=[racc], writes=[row])
                else:
                    S.op("dve", lambda E, h=h, acc=acc: E.tensor_copy(out=ow[:, h, :], in_=acc[:, 0:129]), reads=[racc], writes=[row])

            chk(2)
            S.op("dve", lambda E: E.tensor_scalar(out=den[:, 0, :], in0=cs[:, :, 128], scalar1=1e-30, scalar2=None, op0=ALU.max), reads=[rcs], writes=[rden])
            S.op("dve", lambda E: E.reciprocal(out=den[:, 0, :], in_=den[:, 0, :]), reads=[rden], writes=[rden])
            S.op("dve", lambda E: E.tensor_scalar(out=sblk[:], in0=cs[:, 0, 129:385], scalar1=den[:, 0, 0:1], scalar2=None, op0=ALU.mult), reads=[rcs, rden], writes=[rsblk])
            for h in range(1, 4):
                S.op("dve", lambda E, h=h: E.scalar_tensor_tensor(out=sblk[:], in0=cs[:, h, 129:385], scalar=den[:, 0, h:h + 1], in1=sblk[:],
                                                                  op0=ALU.mult, op1=ALU.add), reads=[rcs, rden, rsblk], writes=[rsblk])
            S.op("dve", lambda E, fa_sb=fa_sb: E.tensor_tensor(out=sblk[:], in0=sblk[:], in1=fa_sb[:], op=ALU.add), reads=[rsblk, rfa], writes=[rsblk])
            chk(3)
            S.op("dve", lambda E: E.max(out=m8[:], in_=sblk[:]), reads=[rsblk], writes=[rm8])
            S.op("dve", lambda E: E.match_replace(out=wk[:], in_to_replace=m8[:], in_values=sblk[:], imm_value=-3.0e38), reads=[rm8, rsblk], writes=[rwk])
            S.op("dve", lambda E: E.max(out=m8[:], in_=wk[:]), reads=[rwk], writes=[rm8])
            S.op("dve", lambda E: E.tensor_reduce(out=thr[:], in_=m8[:], axis=AX.X, op=ALU.min), reads=[rm8], writes=[rthr])
            S.op("dve", lambda E: E.tensor_scalar(out=sel[:], in0=sblk[:], scalar1=thr[:, 0:1], scalar2=-30000.0, op0=ALU.is_lt, op1=ALU.mult), reads=[rsblk, rthr], writes=[rsel])
            for sc in range(nsc):
                S.op("pe", lambda E, sc=sc: E.transpose(out=TB[:, sc, :], in_=sel[:, sc * 128:(sc + 1) * 128], identity=idt[:]), reads=[rsel, rid], writes=[rTB])
            S.op("dve", lambda E, nsc=nsc: E.tensor_copy(out=selT[:, 0:nsc, :], in_=TB[:, 0:nsc, :]), reads=[rTB], writes=[rselT])
            for sc in range(nsc):
                S.op("dve", lambda E, sc=sc: E.tensor_copy(out=selT4[:, sc, :, :], in_=bc(selT[:, sc, :])), reads=[rselT], writes=[rselT4])

            chk(4)
            def sel_pre(kt, sb, rsb):
                S.op("pe", lambda E, kt=kt, sb=sb: E.matmul(sb[:].rearrange("p h q -> p (h q)"), lhsT=E_sb[:, kt % 64, :],
                                                            rhs=selT4[:, kt // 64, :, :].rearrange("p h q -> p (h q)"), start=False, stop=True),
                     reads=[rE, rselT4], writes=[rsb])

            def sel_post(kt, pT, rpT, slot, nkt=nkt):
                if kt >= nkt - 8:
                    j = kt - (nkt - 8)
                    S.op("dve", lambda E: E.tensor_tensor(out=pT[:], in0=pT[:], in1=bc(dm_sb[:, j, :]), op=ALU.mult), reads=[rpT, rdm], writes=[rpT])
            branch(nkt, q_sb, rq, lambda kt: ks_sb[:, kt * 128:(kt + 1) * 128], lambda kt: rks[kt // 32],
                   lambda kt: vs_sb[:, kt, :], lambda kt: rvs[kt // 32], 129, sel_pre, sel_post)
            for h in range(4):
                acc, racc = ACC[h]
                if h % 2 == 0:
                    S.op("act", lambda E, h=h, acc=acc: E.activation(out=os_[:, h, :], in_=acc[:, 0:129], func=AF.Copy), reads=[racc], writes=[ros])
                else:
                    S.op("dve", lambda E, h=h, acc=acc: E.tensor_copy(out=os_[:, h, :], in_=acc[:, 0:129]), reads=[racc], writes=[ros])

            chk(6)
            S.op("dve", lambda E: E.tensor_scalar(out=den[:, 1, :], in0=os_[:, :, 128], scalar1=1e-30, scalar2=None, op0=ALU.max), reads=[ros], writes=[rden])
            S.op("dve", lambda E: E.tensor_scalar(out=den[:, 2, :], in0=ow[:, :, 128], scalar1=1e-30, scalar2=None, op0=ALU.max), reads=[row], writes=[rden])
            S.op("dve", lambda E: E.reciprocal(out=den[:, 1:3, :], in_=den[:, 1:3, :]), reads=[rden], writes=[rden])
            gv = gt_sb[:].rearrange("p (b g h) -> p b g h", b=3, g=4)[:, :, g, :]
            S.op("dve", lambda E, gv=gv: E.tensor_tensor(out=coef[:], in0=den[:], in1=gv, op=ALU.mult), reads=[rden, rgt], writes=[rcoef])

            def cb(i):
                return coef[:, i, :].unsqueeze(2).to_broadcast([128, 4, 128])
            S.op("dve", lambda E: E.tensor_tensor(out=oacc[:], in0=cs[:, :, 0:128], in1=cb(0), op=ALU.mult), reads=[rcs, rcoef], writes=[roacc])
            S.op(POOLENG, lambda E: E.tensor_tensor(out=otmp[:], in0=os_[:, :, 0:128], in1=cb(1), op=ALU.mult), reads=[ros, rcoef], writes=[rotmp])
            S.op("dve", lambda E: E.tensor_tensor(out=oacc[:], in0=oacc[:], in1=otmp[:], op=ALU.add), reads=[roacc, rotmp], writes=[roacc])
            S.op(POOLENG, lambda E: E.tensor_tensor(out=otmp[:], in0=ow[:, :, 0:128], in1=cb(2), op=ALU.mult), reads=[row, rcoef], writes=[rotmp])
            S.op("dve", lambda E, o_sb=o_sb: E.tensor_tensor(out=o_sb[:], in0=oacc[:], in1=otmp[:], op=ALU.add), reads=[roacc, rotmp], writes=[ro])
            c.store("sp", o_d[r, :, g * 512:(g + 1) * 512], o_sb[:].rearrange("p h d -> p (h d)"), ro)
    return c.finish()


def k3_consts():
    E = np.zeros((64, 128, 128), np.float32)
    key = np.arange(128)
    for m in range(64):
        E[m, 2 * m + key // 64, key] = 1.0
    W = np.zeros((1024, 257), np.float32)
    W[:, 0] = 1.0
    for j in range(256):
        for cc, wv in ((4 * j - 1, 1.0), (4 * j, 2.0), (4 * j + 1, 2.0), (4 * j + 2, 2.0), (4 * j + 3, 1.0)):
            if 0 <= cc < 1023:
                W[cc, 1 + j] = wv
    return E.astype(NPBF), W.reshape(8, 128, 257).astype(NPBF)


def k3_core_masks(ci, nround):
    ql = np.arange(128)
    kl = np.arange(128)
    cmask = np.zeros((nround, 2, 128, 128), np.float32)
    wmask = np.zeros((nround, 5, 128, 128), np.float32)
    fadd = np.zeros((nround, 128, 256), np.float32)
    dmask = np.zeros((8, 128, 128), np.float32)
    tri = (kl[:, None] <= ql[None, :]).astype(np.float32)
    for j in range(8):
        dmask[j] = 1.0 if j < ci else (tri if j == ci else 0.0)
    blk = np.arange(256)
    for r in range(nround):
        qb = 8 * r + ci
        qpos = 128 * qb + ql
        chl = qb // 16
        for a, ch in ((1, chl), (0, chl - 1)):
            if ch < 0:
                continue
            cpos = 16 * (128 * ch + kl) + 31
            cmask[r, a] = (cpos[:, None] <= qpos[None, :]) & ((128 * ch + kl) < 1023)[:, None]
        for j in range(5):
            kt = qb - 4 + j
            if kt < 0:
                continue
            kpos = 128 * kt + kl
            diff = qpos[None, :] - kpos[:, None]
            wmask[r, j] = (diff >= 0) & (diff < 512)
        cur = qpos // 64
        valid = blk[None, :] * 64 <= qpos[:, None]
        fa = np.where(valid, 0.0, -1e30).astype(np.float32)
        rows = np.arange(128)
        fa[:, 0] = 1e9
        m1 = cur - 1 >= 1
        fa[rows[m1], (cur - 1)[m1]] = 4e9
        fa[rows, cur] = np.where(cur == 0, 1e9, 2e9)
        fadd[r] = fa
    return cmask.astype(NPBF), dmask.astype(NPBF), wmask.astype(NPBF), fadd


def k3_inputs(ci, nround, qT_full, ksT_full, vs_full, kcT_full, vc_full, kwT_full, vw_full, gates_full, consts):
    E, W, ident = consts
    qT = np.zeros((nround, 16, 128, 128), NPBF)
    kwT = np.zeros((nround, 4, 128, 640), NPBF)
    vw = np.zeros((nround, 640, 4, 128), NPBF)
    gates = np.zeros((nround, 128, 48), np.float32)
    for r in range(nround):
        qb = 8 * r + ci
        qT[r] = qT_full[:, :, 128 * qb:128 * qb + 128]
        lo = 128 * (qb - 4)
        hi = 128 * (qb + 1)
        s0 = max(lo, 0)
        kwT[r, :, :, s0 - lo:] = kwT_full[:, :, s0:hi]
        vw[r, s0 - lo:] = vw_full[s0:hi]
        gates[r] = gates_full[128 * qb:128 * qb + 128]
    cmask, dmask, wmask, fadd = k3_core_masks(ci, nround)
    return {"qT": qT, "ksT": ksT_full, "vs": vs_full, "kcT": kcT_full, "vc": vc_full, "kwT": kwT, "vw": vw,
            "gates": gates, "cmask": cmask, "dmask": dmask, "wmask": wmask, "fadd": fadd,
            "Econst": E, "Wones": W, "ident": ident}


def build_kb(mode, ntile=16):
    c = Ctx()
    S = c.S
    T = ntile * 128
    xin = c.din("xin", [T, D], F32)
    ident = c.din("ident", [128, 128], BF16)
    mem = c.din("mem", [256, D], F32)
    g_kv = c.din("g_kv", [D], F32)
    g_q = c.din("g_q", [D], F32)
    wq_d = c.din("wq", [D, 512], F32)
    wk_d = c.din("wk", [D, 512], F32)
    wv_d = c.din("wv", [D, 512], F32)
    wo_d = c.din("wo", [512, D], F32)
    hout = c.dout("hout", [T, D], F32)
    if mode == "nsa":
        o_d = c.din("o", [T, D], BF16)
        wout_d = c.din("w_out", [D, D], F32)
        wbig, rwbig = c.sb("wbig", [128, 16, D], BF16)
        c.load("pool", wbig[:, 0:8, :], wout_d[0:1024, :].rearrange("(kc p) n -> p kc n", p=128), rwbig)
        c.load("pool", wbig[:, 8:16, :], wout_d[1024:2048, :].rearrange("(kc p) n -> p kc n", p=128), rwbig)
        o_sbs = [c.sb("o_in%d" % i, [128, D], BF16) for i in range(2)]
    else:
        halo = c.din("halo", [128, D], F32)
        g_mix = c.din("g_mix", [D], F32)
        Bm_d = c.din("Bm", [128, 3, 4, 128], BF16)
        wp_d = c.din("wp", [4, 512, 512], F32)
        bp_d = c.din("bp", [D], F32)
        sc_d = c.din("sc", [D], F32)
        wp, rwp = c.sb("wp_sb", [128, 4, 4, 512], BF16)
        for gi in range(4):
            c.load("pool", wp[:, gi, :, :], wp_d[gi].rearrange("(cc p) e -> p cc e", p=128), rwp)
        Bm, rBm = c.sb("Bm_sb", [128, 3, 4, 128], BF16)
        c.load("sp", Bm[:], Bm_d, rBm)
        gm, rgm = c.sb("gm", [128, D], F32)
        bt, rbt = c.sb("bt", [128, D], F32)
        sct, rsct = c.sb("sct", [128, D], F32)
        c.load("sp", gm[:], g_mix.partition_broadcast(128), rgm)
        c.load("sp", bt[:], bp_d.partition_broadcast(128), rbt)
        c.load("sp", sct[:], sc_d.partition_broadcast(128), rsct)
        us = [c.sb("upool%d" % i, [128, D], BF16) for i in range(2)]
        dT, rdT = c.sb("dT", [128, 16, 128], BF16)
        zt, rzt = c.sb("zt", [128, 512], F32)

    idt, rid = c.sb("idt", [128, 128], BF16)
    c.load("sp", idt[:], ident, rid)
    wq, rwq = c.sb("wq_sb", [128, 16, 512], BF16)
    wkv, rwkv = c.sb("wkv_sb", [128, 16, 512], BF16)
    wo, rwo = c.sb("wo_sb", [128, 4, D], BF16)
    c.load("pool", wq[:], wq_d.rearrange("(kc p) n -> p kc n", p=128), rwq)
    c.load("pool", wkv[:], wk_d.rearrange("(kc p) n -> p kc n", p=128), rwkv)
    c.load("pool", wo[:], wo_d.rearrange("(kc p) n -> p kc n", p=128), rwo)
    gA, rgA = c.sb("gA", [128, D], F32)
    c.load("sp", gA[:], g_kv.partition_broadcast(128), rgA)
    xt, rx = c.sb("xt", [128, D], F32)
    h1, rh1 = c.sb("h1", [128, D], F32)
    junk, rj = c.sb("junk", [128, D], BF16)
    ss, rss = c.sb("ss", [128, 2], F32)
    u, ru = c.sb("u", [128, D], BF16)
    aT, raT = c.sb("aT", [128, 16, 128], BF16)
    memnT, rmT = c.sb("memnT", [128, 16, 256], BF16)
    kmT, rkmT = c.sb("kmT", [128, 4, 256], BF16)
    vm, rvm = c.sb("vm", [128, 2, 4, 129], BF16)
    kq, rkq = c.sb("kq", [128, 512], BF16)
    qmT, rqmT = c.sb("qmT", [128, 4, 128], BF16)
    pm, rpm = c.sb("pm", [128, 2, 4, 128], BF16)
    om32, rom32 = c.sb("om32", [128, 4, 129], F32)
    rdn, rrdn = c.sb("rdn", [128, 4], F32)
    om, rom = c.sb("om", [128, 4, 128], BF16)
    omT, romT = c.sb("omT", [128, 4, 128], BF16)
    ho, rho = c.sb("ho", [128, D], F32)

    A = [c.ps("A%d" % i, [128, 512], F32) for i in range(4)]
    TB, rTB = c.ps("TB", [128, 4, 128], BF16)
    QB, rQB = c.ps("QB", [128, 512], F32)
    SBk = [c.ps("SBk%d" % i, [128, 4, 128], F32) for i in range(2)]

    S.op("dve", lambda E: E.memset(vm[:, :, :, 128:129], 1.0), writes=[rvm])
    for mc in range(2):
        c.load("sp", xt[:], mem[mc * 128:(mc + 1) * 128, :], rx)
        rmsnorm_tile(c, xt, rx, gA, rgA, u, ru, junk, rj, ss, rss)
        transpose_to(c, u, ru, 16, lambda k0, n, mc=mc: memnT[:, k0:k0 + n, mc * 128:(mc + 1) * 128], rmT, TB, rTB, idt, rid)
    for mc in range(2):
        for kc in range(16):
            S.op("pe", lambda E, kc=kc, mc=mc: E.matmul(QB[:], lhsT=memnT[:, kc, mc * 128:(mc + 1) * 128], rhs=wkv[:, kc, :],
                                                        start=(kc == 0), stop=(kc == 15)), reads=[rmT, rwkv], writes=[rQB])
        S.op("act", lambda E: E.activation(out=kq[:], in_=QB[:], func=AF.Copy), reads=[rQB], writes=[rkq])
        transpose_to(c, kq, rkq, 4, lambda k0, n, mc=mc: kmT[:, k0:k0 + n, mc * 128:(mc + 1) * 128], rkmT, TB, rTB, idt, rid)
    c.load("pool", wkv[:], wv_d.rearrange("(kc p) n -> p kc n", p=128), rwkv)
    for mc in range(2):
        for kc in range(16):
            S.op("pe", lambda E, kc=kc, mc=mc: E.matmul(QB[:], lhsT=memnT[:, kc, mc * 128:(mc + 1) * 128], rhs=wkv[:, kc, :],
                                                        start=(kc == 0), stop=(kc == 15)), reads=[rmT, rwkv], writes=[rQB])
        S.op("act", lambda E, mc=mc: E.activation(out=vm[:, mc, :, 0:128], in_=QB[:].rearrange("p (h d) -> p h d", h=4), func=AF.Copy),
             reads=[rQB], writes=[rvm])
    c.load("sp", gA[:], g_q.partition_broadcast(128), rgA)

    if mode == "pool":
        c.load("sp", xt[:], halo, rx)
        rmsnorm_tile(c, xt, rx, gm, rgm, us[1][0], us[1][1], junk, rj, ss, rss)

    for tt in range(ntile):
        c.load("sp", xt[:], xin[tt * 128:(tt + 1) * 128, :], rx)
        if mode == "nsa":
            o_sb, ro = o_sbs[tt % 2]
            c.load("sp", o_sb[:], o_d[tt * 128:(tt + 1) * 128, :], ro)
            transpose_to(c, o_sb, ro, 16, lambda k0, n: aT[:, k0:k0 + n, :], raT, TB, rTB, idt, rid)
            for cb in range(4):
                acc, racc = A[cb]
                for kc in range(16):
                    S.op("pe", lambda E, kc=kc, cb=cb, acc=acc: E.matmul(acc[:], lhsT=aT[:, kc, :], rhs=wbig[:, kc, cb * 512:(cb + 1) * 512],
                                                                        start=(kc == 0), stop=(kc == 15)), reads=[raT, rwbig], writes=[racc])
                S.op("dve", lambda E, cb=cb, acc=acc: E.tensor_tensor(out=h1[:, cb * 512:(cb + 1) * 512], in0=acc[:], in1=xt[:, cb * 512:(cb + 1) * 512], op=ALU.add),
                     reads=[racc, rx], writes=[rh1])
        else:
            ucur, rucur = us[tt % 2]
            uprev, ruprev = us[(tt + 1) % 2]
            rmsnorm_tile(c, xt, rx, gm, rgm, ucur, rucur, junk, rj, ss, rss)
            bsel = 0 if tt == 0 else 1
            for gi in range(4):
                for cc in range(4):
                    fc = gi * 4 + cc
                    S.op("pe", lambda E, fc=fc, cc=cc, gi=gi, ucur=ucur, bsel=bsel: E.matmul(QB[:, cc * 128:(cc + 1) * 128], lhsT=ucur[:, fc * 128:(fc + 1) * 128],
                                                                                rhs=Bm[:, bsel, gi, :], start=True, stop=False), reads=[rucur, rBm], writes=[rQB])
                    S.op("pe", lambda E, fc=fc, cc=cc, gi=gi, uprev=uprev: E.matmul(QB[:, cc * 128:(cc + 1) * 128], lhsT=uprev[:, fc * 128:(fc + 1) * 128],
                                                                                  rhs=Bm[:, 2, gi, :], start=False, stop=True), reads=[ruprev, rBm], writes=[rQB])
                S.op("act", lambda E, gi=gi: E.activation(out=dT[:, gi * 4:gi * 4 + 4, :], in_=QB[:].rearrange("p (c t) -> p c t", c=4), func=AF.Copy),
                     reads=[rQB], writes=[rdT])
            for gi in range(4):
                acc, racc = A[gi]
                for cc in range(4):
                    S.op("pe", lambda E, gi=gi, cc=cc, acc=acc: E.matmul(acc[:], lhsT=dT[:, gi * 4 + cc, :], rhs=wp[:, gi, cc, :], start=(cc == 0), stop=(cc == 3)),
                         reads=[rdT, rwp], writes=[racc])
                sl = slice(gi * 512, (gi + 1) * 512)
                S.op("dve", lambda E, acc=acc, sl=sl: E.tensor_tensor(out=zt[:], in0=acc[:], in1=bt[:, sl], op=ALU.add), reads=[racc, rbt], writes=[rzt])
                S.op("dve", lambda E, sl=sl: E.tensor_tensor(out=zt[:], in0=zt[:], in1=sct[:, sl], op=ALU.mult), reads=[rzt, rsct], writes=[rzt])
                S.op("dve", lambda E, sl=sl: E.tensor_tensor(out=h1[:, sl], in0=zt[:], in1=xt[:, sl], op=ALU.add), reads=[rzt, rx], writes=[rh1])
        rmsnorm_tile(c, h1, rh1, gA, rgA, u, ru, junk, rj, ss, rss)
        transpose_to(c, u, ru, 16, lambda k0, n: aT[:, k0:k0 + n, :], raT, TB, rTB, idt, rid)
        for kc in range(16):
            S.op("pe", lambda E, kc=kc: E.matmul(QB[:], lhsT=aT[:, kc, :], rhs=wq[:, kc, :], start=(kc == 0), stop=(kc == 15)),
                 reads=[raT, rwq], writes=[rQB])
        S.op("act", lambda E: E.activation(out=kq[:], in_=QB[:], func=AF.Copy, scale=128.0 ** -0.5), reads=[rQB], writes=[rkq])
        transpose_to(c, kq, rkq, 4, lambda k0, n: qmT[:, k0:k0 + n, :], rqmT, TB, rTB, idt, rid)
        for mc in range(2):
            sb, rsb = SBk[mc]
            for h in range(4):
                S.op("pe", lambda E, h=h, mc=mc, sb=sb: E.matmul(sb[:, h, :], lhsT=kmT[:, h, mc * 128:(mc + 1) * 128], rhs=qmT[:, h, :], start=True, stop=True),
                     reads=[rkmT, rqmT], writes=[rsb])
            S.op("act", lambda E, mc=mc, sb=sb: E.activation(out=pm[:, mc, :, :], in_=sb[:], func=AF.Exp), reads=[rsb], writes=[rpm])
        for h in range(4):
            acc, racc = A[h]
            for mc in range(2):
                S.op("pe", lambda E, h=h, mc=mc, acc=acc: E.matmul(acc[:, 0:129], lhsT=pm[:, mc, h, :], rhs=vm[:, mc, h, :], start=(mc == 0), stop=(mc == 1)),
                     reads=[rpm, rvm], writes=[racc])
            S.op("act" if h % 2 == 0 else "dve",
                 (lambda E, h=h, acc=acc: E.activation(out=om32[:, h, :], in_=acc[:, 0:129], func=AF.Copy)) if h % 2 == 0 else
                 (lambda E, h=h, acc=acc: E.tensor_copy(out=om32[:, h, :], in_=acc[:, 0:129])), reads=[racc], writes=[rom32])
        S.op("dve", lambda E: E.reciprocal(out=rdn[:], in_=om32[:, :, 128]), reads=[rom32], writes=[rrdn])
        S.op("dve", lambda E: E.tensor_tensor(out=om[:], in0=om32[:, :, 0:128], in1=rdn[:].unsqueeze(2).to_broadcast([128, 4, 128]), op=ALU.mult),
             reads=[rom32, rrdn], writes=[rom])
        transpose_to(c, om[:].rearrange("p h d -> p (h d)"), rom, 4, lambda k0, n: omT[:, k0:k0 + n, :], romT, TB, rTB, idt, rid)
        for cb in range(4):
            acc, racc = A[cb]
            for kc in range(4):
                S.op("pe", lambda E, kc=kc, cb=cb, acc=acc: E.matmul(acc[:], lhsT=omT[:, kc, :], rhs=wo[:, kc, cb * 512:(cb + 1) * 512], start=(kc == 0), stop=(kc == 3)),
                     reads=[romT, rwo], writes=[racc])
            S.op("dve", lambda E, cb=cb, acc=acc: E.tensor_tensor(out=ho[:, cb * 512:(cb + 1) * 512], in0=acc[:], in1=h1[:, cb * 512:(cb + 1) * 512], op=ALU.add),
                 reads=[racc, rh1], writes=[rho])
        c.store("sp", hout[tt * 128:(tt + 1) * 128, :], ho[:], rho)
    return c.finish()


def pool_bmats(first):
    B = np.zeros((128, 3, 4, 128), np.float32)
    s = np.arange(128)[:, None]
    t = np.arange(128)[None, :]
    for gi, w in enumerate((2, 4, 8, 16)):
        gen = ((s > t - w) & (s <= t)) / float(w) - (s == t)
        cnt = np.minimum(t + 1, w).astype(np.float32)
        fst = ((s > t - w) & (s <= t)) / cnt - (s == t)
        B[:, 0, gi] = fst if first else gen
        B[:, 1, gi] = gen
        B[:, 2, gi] = ((s - 128) > (t - w)) / float(w)
    return B.astype(NPBF)


DFF = 5632
import os as _os
KF_DBG = int(_os.environ.get('KF_DBG', '0'))
KF_SCR_MIN = int(_os.environ.get('KF_SCR_MIN', '8'))


def build_kf(mode, ntile=16):
    c = Ctx()
    S = c.S
    T = ntile * 128
    nst = ntile // 4
    hin = c.din("hin", [T, D], F32)
    g = c.din("g", [D], F32)
    wg_d = c.din("wg", [D, DFF], F32)
    wu_d = c.din("wu", [D, DFF], F32)
    wd_d = c.din("wd", [DFF, D], F32)
    identf = c.din("identf", [128, 128], F32)
    y_d = c.dout("y", [T, D], F32)
    moe = mode == "moe"
    if moe:
        onehot_d = c.din("onehot", [128, 8], F32)
        oh, roh = c.sb("oh", [128, 8], F32)
        c.load("sp", oh[:], onehot_d, roh)
        routerT_d = c.din("routerT", [8, D], F32)
        rtbs = [c.sb("rtb%d" % i, [128, D], F32) for i in range(2)]
        j32s = [c.sb("j32_%d" % i, [128, D], F32) for i in range(2)]
        lg, rlg = c.sb("lg", [128, 8], F32)
        tm, rtm = c.sb("tm", [128, 8], F32)
        sm, rsm = c.sb("sm", [128, 8], F32)

    idf, ridf = c.sb("idf", [128, 128], F32)
    c.load("sp", idf[:], identf, ridf)
    gt, rgt = c.sb("gt", [128, D], F32)
    c.load("sp", gt[:], g.partition_broadcast(128), rgt)
    xt, rx = c.sb("xt", [128, D], F32)
    junk, rj = c.sb("junk", [128, D], BF16)
    ss, rss = c.sb("ss", [128, 2], F32)
    u32, ru32 = c.sb("u32", [128, D], F32)
    uTs = [c.sb("uT%d" % i, [128, 16, 512], BF16) for i in range(2 if nst > 1 else 1)] * (1 if nst > 1 else 2)
    hT, rhT = c.sb("hT", [128, 22, 512], BF16)
    wgs = [c.sb("wg%d" % i, [128, 16, 256], BF16) for i in range(2)]
    wus = [c.sb("wu%d" % i, [128, 16, 256], BF16) for i in range(2)]
    wds = [c.sb("wd%d" % i, [128, 22, 256], BF16) for i in range(2)]
    ys = [c.sb("y%d" % i, [128, D], F32) for i in range(4)]
    sgs = [c.sb("sg%d" % i, [128, 512], F32) for i in range(2)]
    gwss = [c.sb("gws%d" % i, [128, 4], F32) for i in range(2)]

    TB, rTB = c.ps("TB32", [128, 4, 128], F32)
    GB = [c.ps("GB%d" % i, [128, 512], F32) for i in range(2)]
    UB = [c.ps("UB%d" % i, [128, 512], F32) for i in range(2)]
    AB = [c.ps("AB%d" % i, [128, 256], F32) for i in range(2)]
    LB, rLB = c.ps("LB", [128, 8], F32)

    if not moe or KF_DBG >= 1:
        for gws, rgws in gwss:
            S.op("dve", lambda E, gws=gws: E.memset(gws[:], 1.0), writes=[rgws])
    wi = 0
    wdi = 0
    use_scr = nst >= KF_SCR_MIN
    rscr = Res()
    if use_scr:
        wg_s = c.nc.dram_tensor("wg_s", [2, 22, 128, 16 * 256], BF16).ap()
        wd_s = c.nc.dram_tensor("wd_s", [2, 8, 128, 22 * 256], BF16).ap()
        for fbg in range(22):
            f0 = fbg * 256
            wg, rwg = wgs[fbg % 2]
            wu, rwu = wus[fbg % 2]
            c.load("pool", wg[:], wg_d[:, f0:f0 + 256].rearrange("(kc p) n -> p kc n", p=128), rwg)
            S.dma("sp", wg_s[0, fbg], wg[:].rearrange("p k n -> p (k n)"), reads=[rwg], writes=[rscr])
            c.load("pool", wu[:], wu_d[:, f0:f0 + 256].rearrange("(kc p) n -> p kc n", p=128), rwu)
            S.dma("sp", wg_s[1, fbg], wu[:].rearrange("p k n -> p (k n)"), reads=[rwu], writes=[rscr])
        for half in range(2):
            for db in range(8):
                wd, rwd = wds[(half * 8 + db) % len(wds)]
                c.load("pool", wd[:], wd_d[half * 2816:(half + 1) * 2816, db * 256:(db + 1) * 256].rearrange("(j p) n -> p j n", p=128), rwd)
                S.dma("sp", wd_s[half, db], wd[:].rearrange("p j n -> p (j n)"), reads=[rwd], writes=[rscr])
    def phaseA_tile(st, tt):
        uT, ruT = uTs[st % 2]
        gws, rgws = gwss[st % 2]
        row = (st * 4 + tt) * 128
        c.load("sp", xt[:], hin[row:row + 128, :], rx)
        rmsnorm_tile(c, xt, rx, gt, rgt, u32, ru32, junk, rj, ss, rss)
        for k0 in range(0, 16, 4):
            for k in range(4):
                S.op("pe", lambda E, k=k, k0=k0: E.transpose(out=TB[:, k, :], in_=u32[:, (k0 + k) * 128:(k0 + k + 1) * 128], identity=idf[:]),
                     reads=[ru32, ridf], writes=[rTB])
            S.op("act", lambda E, k0=k0, tt=tt: E.activation(out=uT[:, k0:k0 + 4, tt * 128:(tt + 1) * 128], in_=TB[:], func=AF.Copy), reads=[rTB], writes=[ruT])
        if moe and KF_DBG < 1:
            for ex in range(8):
                rb_, rrb_ = rtbs[ex % 2]
                c.load("sp", rb_[:], routerT_d[ex].partition_broadcast(128), rrb_)
                jb, rjb = j32s[ex % 2]
                S.op("dve", lambda E, rb_=rb_, jb=jb: E.tensor_tensor(out=jb[:], in0=u32[:], in1=rb_[:], op=ALU.mult), reads=[ru32, rrb_], writes=[rjb])
                S.op("act", lambda E, ex=ex, jb=jb: E.activation(out=jb[:], in_=jb[:], func=AF.Copy, accum_out=lg[:, ex:ex + 1]), reads=[rjb], writes=[rjb, rlg])
            S.op("dve", lambda E: E.tensor_reduce(out=sm[:, 0:1], in_=lg[:], axis=AX.X, op=ALU.max), reads=[rlg], writes=[rsm])
            S.op("dve", lambda E: E.tensor_scalar(out=tm[:], in0=lg[:], scalar1=sm[:, 0:1], scalar2=-1e30, op0=ALU.is_ge, op1=ALU.mult), reads=[rlg, rsm], writes=[rtm])
            S.op("dve", lambda E: E.tensor_tensor(out=tm[:], in0=tm[:], in1=lg[:], op=ALU.add), reads=[rtm, rlg], writes=[rtm])
            S.op("dve", lambda E: E.tensor_reduce(out=sm[:, 1:2], in_=tm[:], axis=AX.X, op=ALU.max), reads=[rtm], writes=[rsm])
            S.op("dve", lambda E: E.tensor_tensor(out=tm[:], in0=lg[:], in1=oh[:], op=ALU.mult), reads=[rlg, roh], writes=[rtm])
            S.op("dve", lambda E: E.tensor_reduce(out=sm[:, 2:3], in_=tm[:], axis=AX.X, op=ALU.add), reads=[rtm], writes=[rsm])
            S.op("dve", lambda E: E.tensor_scalar(out=sm[:, 3:5], in0=sm[:, 1:3], scalar1=sm[:, 0:1], scalar2=None, op0=ALU.subtract), reads=[rsm], writes=[rsm])
            S.op("act", lambda E: E.activation(out=sm[:, 3:5], in_=sm[:, 3:5], func=AF.Exp), reads=[rsm], writes=[rsm])
            S.op("dve", lambda E: E.tensor_scalar(out=sm[:, 3:4], in0=sm[:, 3:4], scalar1=1.0, scalar2=None, op0=ALU.add), reads=[rsm], writes=[rsm])
            S.op("dve", lambda E: E.reciprocal(out=sm[:, 3:4], in_=sm[:, 3:4]), reads=[rsm], writes=[rsm])
            S.op("dve", lambda E: E.tensor_tensor(out=sm[:, 5:6], in0=sm[:, 2:3], in1=sm[:, 1:2], op=ALU.is_ge), reads=[rsm], writes=[rsm])
            S.op("dve", lambda E: E.tensor_tensor(out=sm[:, 4:5], in0=sm[:, 4:5], in1=sm[:, 3:4], op=ALU.mult), reads=[rsm], writes=[rsm])
            S.op("dve", lambda E, tt=tt: E.tensor_tensor(out=gws[:, tt:tt + 1], in0=sm[:, 4:5], in1=sm[:, 5:6], op=ALU.mult), reads=[rsm], writes=[rgws])

    for tt in range(4):
        phaseA_tile(0, tt)
    for st in range(nst):
        uT, ruT = uTs[st % 2]
        gws, rgws = gwss[st % 2]
        for tt in range(4):
            row = (st * 4 + tt) * 128
            y_sb, ry = ys[tt]
            if moe:
                S.op("dve", lambda E, y_sb=y_sb: E.memset(y_sb[:], 0.0), writes=[ry])
            else:
                c.load("sp", y_sb[:], hin[row:row + 128, :], ry)
        for half in range(2):
            for fb in range(11):
                f0 = half * 2816 + fb * 256
                wg, rwg = wgs[wi % 2]
                wu, rwu = wus[wi % 2]
                wi += 1
                if use_scr:
                    S.dma("pool", wg[:].rearrange("p k n -> p (k n)"), wg_s[0, half * 11 + fb], reads=[rscr], writes=[rwg])
                    S.dma("pool", wu[:].rearrange("p k n -> p (k n)"), wg_s[1, half * 11 + fb], reads=[rscr], writes=[rwu])
                else:
                    c.load("pool", wg[:], wg_d[:, f0:f0 + 256].rearrange("(kc p) n -> p kc n", p=128), rwg)
                    c.load("pool", wu[:], wu_d[:, f0:f0 + 256].rearrange("(kc p) n -> p kc n", p=128), rwu)
                for j in range(2):
                    gb, rgb = GB[j]
                    ub, rub = UB[j]
                    sg, rsg = sgs[j]
                    for kc in range(16):
                        S.op("pe", lambda E, kc=kc, j=j, gb=gb, wg=wg, uT=uT: E.matmul(gb[:], lhsT=wg[:, kc, j * 128:(j + 1) * 128], rhs=uT[:, kc, :],
                                                                              start=(kc == 0), stop=(kc == 15)), reads=[rwg, ruT], writes=[rgb])
                    for kc in range(16):
                        S.op("pe", lambda E, kc=kc, j=j, ub=ub, wu=wu, uT=uT: E.matmul(ub[:], lhsT=wu[:, kc, j * 128:(j + 1) * 128], rhs=uT[:, kc, :],
                                                                              start=(kc == 0), stop=(kc == 15)), reads=[rwu, ruT], writes=[rub])
                    S.op("act", lambda E, sg=sg, gb=gb: E.activation(out=sg[:], in_=gb[:], func=AF.Silu), reads=[rgb], writes=[rsg])
                    S.op("dve", lambda E, sg=sg, ub=ub, fb=fb, j=j: E.tensor_tensor(out=hT[:, fb * 2 + j, :], in0=sg[:], in1=ub[:], op=ALU.mult),
                         reads=[rsg, rub], writes=[rhT])
                if half == 0 and st + 1 < nst and fb in (1, 3, 5, 7):
                    phaseA_tile(st + 1, (fb - 1) // 2)
            for db in range(8):
                wd, rwd = wds[wdi % len(wds)]
                wdi += 1
                if use_scr:
                    S.dma("pool", wd[:].rearrange("p j n -> p (j n)"), wd_s[half, db], reads=[rscr], writes=[rwd])
                else:
                    c.load("pool", wd[:], wd_d[half * 2816:(half + 1) * 2816, db * 256:(db + 1) * 256].rearrange("(j p) n -> p j n", p=128), rwd)
                for tt in range(4):
                    ab, rab = AB[tt % 2]
                    y_sb, ry = ys[tt]
                    for j in range(22):
                        S.op("pe", lambda E, j=j, tt=tt, ab=ab, wd=wd: E.matmul(ab[:], lhsT=hT[:, j, tt * 128:(tt + 1) * 128], rhs=wd[:, j, :],
                                                                              start=(j == 0), stop=(j == 21)), reads=[rhT, rwd], writes=[rab])
                    S.op("dve", lambda E, ab=ab, y_sb=y_sb, tt=tt, db=db, gws=gws: E.scalar_tensor_tensor(
                        out=y_sb[:, db * 256:(db + 1) * 256], in0=ab[:], scalar=gws[:, tt:tt + 1], in1=y_sb[:, db * 256:(db + 1) * 256],
                        op0=ALU.mult, op1=ALU.add), reads=[rab, rgws, ry], writes=[ry])
        for tt in range(4):
            row = (st * 4 + tt) * 128
            y_sb, ry = ys[tt]
            c.store("sp", y_d[row:row + 128, :], y_sb[:], ry)
    return c.finish()


def build_kz(ntile=16):
    c = Ctx()
    S = c.S
    T = ntile * 128
    h_d = c.din("h", [T, D], F32)
    parts = c.din("parts", [8, T, D], F32)
    g = c.din("g", [D], F32)
    out = c.dout("out", [T, D], F32)
    gt, rgt = c.sb("gt", [128, D], F32)
    c.load("sp", gt[:], g.partition_broadcast(128), rgt)
    accs = [c.sb("acc%d" % i, [128, D], F32) for i in range(2)]
    pts = [c.sb("pt%d" % i, [128, D], F32) for i in range(3)]
    junk, rj = c.sb("junk", [128, D], BF16)
    ss, rss = c.sb("ss", [128, 2], F32)
    outs = [c.sb("o%d" % i, [128, D], F32) for i in range(2)]
    k = 0
    for tt in range(ntile):
        acc, racc = accs[tt % 2]
        o_sb, ro = outs[tt % 2]
        c.load("sp", acc[:], h_d[tt * 128:(tt + 1) * 128, :], racc)
        for e in range(8):
            pt, rpt = pts[k % 3]
            k += 1
            c.load("sp", pt[:], parts[e, tt * 128:(tt + 1) * 128, :], rpt)
            S.op("dve", lambda E, acc=acc, pt=pt: E.tensor_tensor(out=acc[:], in0=acc[:], in1=pt[:], op=ALU.add), reads=[racc, rpt], writes=[racc])
        rmsnorm_tile(c, acc, racc, gt, rgt, o_sb, ro, junk, rj, ss, rss)
        c.store("sp", out[tt * 128:(tt + 1) * 128, :], o_sb[:], ro)
    return c.finish()


def _run(nc, in_maps):
    res = run_bass_kernel_spmd(nc, in_maps, core_ids=list(range(NCORES)))
    et = getattr(res, "exec_time_ns", None)
    if et is not None:
        print("[launch exec_time_ns]", et, flush=True)
    return res.results


def kernel(**inp):
    f32 = np.float32
    x = np.ascontiguousarray(inp["x"][0], dtype=f32)
    ident = np.eye(128).astype(NPBF)
    identf = np.eye(128, dtype=f32)
    sl = lambda ci: slice(ci * TPC, (ci + 1) * TPC)
    in_maps = []
    for ci in range(NCORES):
        pos = np.arange(ci * TPC, (ci + 1) * TPC)
        cq, sq = rope_tables(pos, 128 ** -0.5)
        ck, sk = rope_tables(pos)
        in_maps.append({"x": x[sl(ci)], "g": inp["norm_mix"][0], "w": inp["nsa_w_in"][0], "gb": inp["nsa_gate_b"][0],
                        "ropet": np.stack([cq, sq, ck, sk]), "ident": ident})
    r1 = _run(build_k1(16), in_maps)
    qT_full = np.zeros((16, 128, SEQ), NPBF)
    feat = np.zeros((4, 4, 128, SEQ), NPBF)
    vs_full = np.zeros((SEQ, 4, 128), NPBF)
    vw_full = np.zeros((SEQ, 4, 128), NPBF)
    gates_full = np.zeros((SEQ, 48), f32)
    for ci in range(NCORES):
        fT = r1[ci]["fT"]
        qT_full[:, :, sl(ci)] = fT[0:4].reshape(16, 128, TPC)
        feat[:, :, :, sl(ci)] = fT[4:8]
        vs_full[sl(ci)] = r1[ci]["vtok"][0].reshape(TPC, 4, 128)
        vw_full[sl(ci)] = r1[ci]["vtok"][1].reshape(TPC, 4, 128)
        gates_full[sl(ci)] = r1[ci]["gates"]
    full = np.zeros((2, 4, 128, SEQ + 16), NPBF)
    full[0, :, :, :SEQ] = feat[0]
    full[1, :, :, :SEQ] = feat[1]
    w1 = np.stack([inp["nsa_cmp_k_w1"][0], inp["nsa_cmp_v_w1"][0]])
    w2 = np.stack([inp["nsa_cmp_k_w2"][0], inp["nsa_cmp_v_w2"][0]])
    peT = np.stack([np.ascontiguousarray(inp["nsa_pe_k"][0].T), np.ascontiguousarray(inp["nsa_pe_v"][0].T)])
    in_maps = []
    for ci in range(NCORES):
        cpos = (np.arange(128) + 128 * ci) * 16 + 31
        cc, sc = rope_tables(cpos)
        in_maps.append({"xT": np.ascontiguousarray(full[:, :, :, ci * TPC:ci * TPC + TPC + 16]), "w1": w1, "w2": w2, "peT": peT,
                        "ropec": np.stack([cc, sc]), "ident": ident})
    r2 = _run(build_k2(), in_maps)
    kcT_full = np.zeros((4, 128, 1024), NPBF)
    vc_full = np.zeros((1024, 4, 128), NPBF)
    for ci in range(NCORES):
        kcT_full[:, :, ci * 128:(ci + 1) * 128] = r2[ci]["kcT"]
        vc_full[ci * 128:(ci + 1) * 128] = r2[ci]["vc"]
    E, W = k3_consts()
    ksT_full = np.ascontiguousarray(feat[2])
    kwT_full = np.ascontiguousarray(feat[3])
    in_maps = [k3_inputs(ci, 16, qT_full, ksT_full, vs_full, kcT_full, vc_full, kwT_full, vw_full, gates_full, (E, W, ident))
               for ci in range(NCORES)]
    r3 = _run(build_k3(16), in_maps)
    o_full = np.zeros((SEQ, D), NPBF)
    for ci in range(NCORES):
        for r in range(16):
            qb = 8 * r + ci
            o_full[128 * qb:128 * qb + 128] = r3[ci]["o"][r]
    def memw(L):
        return {"mem": inp["mem"][0], "g_kv": inp["norm_mem_kv"][L], "g_q": inp["norm_mem_q"][L], "wq": inp["mem_wq"][L],
                "wk": inp["mem_wk"][L], "wv": inp["mem_wv"][L], "wo": inp["mem_wo"][L], "ident": ident}
    in_maps = [dict(memw(0), xin=x[sl(ci)], o=o_full[sl(ci)], w_out=inp["nsa_w_out"][0]) for ci in range(NCORES)]
    r4 = _run(build_kb("nsa", 16), in_maps)
    h2 = np.concatenate([r4[ci]["hout"] for ci in range(NCORES)], axis=0)
    in_maps = [{"hin": h2[sl(ci)], "g": inp["norm_ffn"][0], "wg": inp["ffn_w_gate"][0], "wu": inp["ffn_w_up"][0],
                "wd": inp["ffn_w_down"][0], "identf": identf} for ci in range(NCORES)]
    r5 = _run(build_kf("dense", 16), in_maps)
    h3 = np.concatenate([r5[ci]["y"] for ci in range(NCORES)], axis=0)
    in_maps = []
    for ci in range(NCORES):
        halo = h3[ci * TPC - 128:ci * TPC] if ci > 0 else np.zeros((128, D), f32)
        in_maps.append(dict(memw(1), xin=h3[sl(ci)], halo=halo, g_mix=inp["norm_mix"][1], Bm=pool_bmats(ci == 0),
                            wp=inp["pool_w"][0], bp=np.ascontiguousarray(inp["pool_b"][0].reshape(-1)), sc=inp["pool_scale"][0]))
    r6 = _run(build_kb("pool", 16), in_maps)
    h5 = np.concatenate([r6[ci]["hout"] for ci in range(NCORES)], axis=0)
    routerT = np.ascontiguousarray(inp["moe_router"][0].T)
    in_maps = []
    for ci in range(NCORES):
        oh = np.zeros((128, 8), f32)
        oh[:, ci] = 1.0
        in_maps.append({"hin": h5, "g": inp["norm_ffn"][1], "wg": inp["moe_w_gate"][0, ci], "wu": inp["moe_w_up"][0, ci],
                        "wd": inp["moe_w_down"][0, ci], "identf": identf, "routerT": routerT, "onehot": oh})
    r7 = _run(build_kf("moe", SEQ // 128), in_maps)
    in_maps = [{"h": h5[sl(ci)], "parts": np.stack([r7[e]["y"][sl(ci)] for e in range(NCORES)]), "g": inp["norm_final"]}
               for ci in range(NCORES)]
    r8 = _run(build_kz(16), in_maps)
    out = np.concatenate([r8[ci]["out"] for ci in range(NCORES)], axis=0)
    return out.reshape(1, SEQ, D).astype(f32)
```

```python
import numpy as np
import concourse.bass as bass
import concourse.mybir as mybir
from concourse.bass_utils import run_bass_kernel_spmd

F32 = mybir.dt.float32
BF16 = mybir.dt.bfloat16
AF = mybir.ActivationFunctionType
ALU = mybir.AluOpType
AX = mybir.AxisListType


class Res:
    __slots__ = ("lw", "rd")

    def __init__(self):
        self.lw = None
        self.rd = {}


class Sched:
    ENG = ("pe", "act", "dve", "pool", "sp")
    NDMA = 6

    def __init__(self, nc, stack):
        self.nc = nc
        self.prog = {e: [] for e in self.ENG}
        self.sems = {}
        for e in ("pe", "act", "dve", "pool"):
            self.sems[e] = stack.enter_context(nc.semaphore("s_" + e))
        self.cnt = {e: 0 for e in self.ENG}
        self.known = {e: {} for e in self.ENG}
        self.dq = {}
        for q in ("sp", "pool", "act"):
            sl = []
            for i in range(self.NDMA):
                k = "d_%s%d" % (q, i)
                self.sems[k] = stack.enter_context(nc.semaphore(k))
                sl.append(k)
            self.dq[q] = [sl, 0]
        self.nwait = 0
        self.off = False
        self.ncc = 0
        self.sems["s_cc"] = stack.enter_context(nc.semaphore("s_cc"))

    def _wait(self, eng, key, val):
        if val <= 0:
            return
        kn = self.known[eng]
        if kn.get(key, 0) >= val:
            return
        kn[key] = val
        sem = self.sems[key]
        self.nwait += 1
        self.prog[eng].append(lambda E, sem=sem, val=val: E.wait_ge(sem, val))

    def _deps(self, eng, reads, writes):
        for r in reads:
            if r.lw is not None:
                self._wait(eng, *r.lw)
        for w in writes:
            if w.lw is not None:
                self._wait(eng, *w.lw)
            for k, v in w.rd.items():
                self._wait(eng, k, v)

    def op(self, eng, fn, reads=(), writes=()):
        if self.off:
            return
        self._deps(eng, reads, writes)
        self.cnt[eng] += 1
        idx = self.cnt[eng]
        sem = self.sems[eng]
        self.prog[eng].append(lambda E, fn=fn, sem=sem: fn(E).then_inc(sem, 1))
        if eng == "pe":
            self.known[eng][eng] = idx
        for r in reads:
            r.rd[eng] = idx
        for w in writes:
            w.lw = (eng, idx)
            w.rd = {}

    def dma(self, q, out, in_, reads=(), writes=()):
        if self.off:
            return None
        sl, n = self.dq[q]
        key = sl[n % self.NDMA]
        val = 16 * (n // self.NDMA + 1)
        self.dq[q][1] = n + 1
        self._wait(q, key, val - 16)
        self._deps(q, reads, writes)
        sem = self.sems[key]
        self.prog[q].append(lambda E, out=out, in_=in_, sem=sem: E.dma_start(out=out, in_=in_).then_inc(sem, 16))
        for r in reads:
            r.rd[key] = val
        for w in writes:
            w.lw = (key, val)
            w.rd = {}
        return key, val

    def coll(self, kind, op, ins, outs, reads=(), writes=()):
        if self.off:
            return None
        q = "pool"
        key = "s_cc"
        self.ncc += 1
        val = self.ncc
        self._wait(q, key, val - 1)
        self._deps(q, reads, writes)
        sem = self.sems[key]
        self.prog[q].append(lambda E, sem=sem: E.collective_compute(kind, op, replica_groups=[list(range(8))],
                                                                    ins=[a_.opt() for a_ in ins], outs=[a_.opt() for a_ in outs]).then_inc(sem))
        for r in reads:
            r.rd[key] = val
        for w in writes:
            w.lw = (key, val)
            w.rd = {}
        return key, val

    def barrier(self):
        tg = {e: self.cnt[e] for e in ("pe", "act", "dve", "pool")}
        tg["s_cc"] = self.ncc
        for q, (sl, n) in self.dq.items():
            for i, key in enumerate(sl):
                tg[key] = 16 * ((n - i + self.NDMA - 1) // self.NDMA) if n > i else 0
        for eng in (self.BENG if hasattr(self, 'BENG') else self.ENG):
            for k, v in tg.items():
                self._wait(eng, k, v)

    def finish(self, pending):
        for key, val in pending:
            self._wait("sp", key, val)
        nc = self.nc
        prog = self.prog
        self.prog = {e: [] for e in self.ENG}
        with nc.Block() as block:
            @block.sync
            def _(E):
                for f in prog["sp"]:
                    f(E)

            @block.tensor
            def _(E):
                for f in prog["pe"]:
                    f(E)

            @block.scalar
            def _(E):
                for f in prog["act"]:
                    f(E)

            @block.vector
            def _(E):
                for f in prog["dve"]:
                    f(E)

            @block.gpsimd
            def _(E):
                for f in prog["pool"]:
                    f(E)


import ml_dtypes
from contextlib import ExitStack

NPBF = ml_dtypes.bfloat16
NCORES = 8
D = 2048
SEQ = 16384
TPC = SEQ // NCORES
EPS = 1e-6


class Ctx:
    def __init__(self):
        self.nc = bass.Bass("TRN2", target_bir_lowering=False)
        self.st = ExitStack()
        self.pst = ExitStack()
        self.S = Sched(self.nc, self.st)
        self.n = 0
        self.out_pending = []
        self.bind = {}
        self.prefix = ""
        self.chained = False

    def din(self, name, shape, dt):
        if name in self.bind:
            return self.bind[name]
        return self.nc.dram_tensor(self.prefix + name, list(shape), dt, kind="ExternalInput").ap()

    def dout(self, name, shape, dt):
        if name in self.bind:
            return self.bind[name]
        return self.nc.dram_tensor(self.prefix + name, list(shape), dt, kind="ExternalOutput").ap()

    def scratch(self, name, shape, dt):
        return self.nc.dram_tensor(self.prefix + name, list(shape), dt).ap()

    def sb(self, name, shape, dt):
        t = self.pst.enter_context(self.nc.sbuf_tensor(self.prefix + name, list(shape), dt))
        return t, Res()

    def ps(self, name, shape, dt=F32):
        t = self.pst.enter_context(self.nc.psum_tensor(self.prefix + name, list(shape), dt))
        return t, Res()

    def load(self, q, out, in_, w):
        self.S.dma(q, out, in_, writes=[w])

    def store(self, q, out, in_, r):
        kv = self.S.dma(q, out, in_, reads=[r])
        if kv is not None:
            self.out_pending.append(kv)

    def next_phase(self, prefix, bind):
        self.S.barrier()
        self.S.finish(self.out_pending)
        self.out_pending = []
        self.pst.close()
        self.pst = ExitStack()
        self.prefix = prefix
        self.bind = bind

    def finish(self):
        if self.chained:
            return None
        self.S.finish(self.out_pending)
        self.pst.close()
        self.st.close()
        return self.nc


def rmsnorm_tile(c, xt, rx, gt, rg, u, ru, junk, rj, ss, rss):
    S = c.S
    S.op("act", lambda E: E.activation(out=junk[:], in_=xt[:], func=AF.Square, accum_out=ss[:, 0:1]),
         reads=[rx], writes=[rj, rss])
    S.op("dve", lambda E: E.tensor_scalar(out=ss[:, 1:2], in0=ss[:, 0:1], scalar1=1.0 / D, scalar2=EPS,
                                          op0=ALU.mult, op1=ALU.add), reads=[rss], writes=[rss])
    S.op("act", lambda E: E.activation(out=ss[:, 1:2], in_=ss[:, 1:2], func=AF.Sqrt), reads=[rss], writes=[rss])
    S.op("dve", lambda E: E.reciprocal(out=ss[:, 1:2], in_=ss[:, 1:2]), reads=[rss], writes=[rss])
    S.op("dve", lambda E: E.scalar_tensor_tensor(out=u[:], in0=xt[:], scalar=ss[:, 1:2], in1=gt[:],
                                                 op0=ALU.mult, op1=ALU.mult), reads=[rx, rss, rg], writes=[ru])


def transpose_to(c, src, rsrc, nchunk, dst_fn, rdst, pT, rpT, idt, rid, eng_cycle=("dve", "act")):
    S = c.S
    for k0 in range(0, nchunk, 4):
        n = min(4, nchunk - k0)
        for k in range(n):
            S.op("pe", lambda E, k=k, k0=k0: E.transpose(out=pT[:, k, :], in_=src[:, (k0 + k) * 128:(k0 + k + 1) * 128],
                                                      identity=idt[:]), reads=[rsrc, rid], writes=[rpT])
        eng = eng_cycle[(k0 // 4) % len(eng_cycle)]
        if eng == "act":
            S.op("act", lambda E, k0=k0, n=n: E.activation(out=dst_fn(k0, n), in_=pT[:, 0:n, :], func=AF.Copy),
                 reads=[rpT], writes=[rdst])
        else:
            S.op(eng, lambda E, k0=k0, n=n: E.tensor_copy(out=dst_fn(k0, n), in_=pT[:, 0:n, :]),
                 reads=[rpT], writes=[rdst])


NSA_W = 5168


def build_k1(ntile=16):
    c = Ctx()
    S = c.S
    T = ntile * 128
    x = c.din("x", [T, D], F32)
    g = c.din("g", [D], F32)
    w = c.din("w", [D, NSA_W], F32)
    gb = c.din("gb", [48], F32)
    ropet = c.din("ropet", [4, T, 64], F32)
    ident = c.din("ident", [128, 128], BF16)
    fT = c.dout("fT", [8, 4, 128, T], BF16)
    vtok = c.dout("vtok", [2, T, 512], BF16)
    gates = c.dout("gates", [T, 48], F32)

    uT, ruT = c.sb("uT", [128, 16, T], BF16)
    gt, rgt = c.sb("gt", [128, D], F32)
    idt, rid = c.sb("idt", [128, 128], BF16)
    gbt, rgb = c.sb("gbt", [128, 48], F32)
    rt, rrt = c.sb("rt", [128, 4, ntile, 64], F32)
    xts = [c.sb("xt%d" % i, [128, D], F32) for i in range(2)]
    junk, rj = c.sb("junk", [128, D], BF16)
    ss, rss = c.sb("ss", [128, 2], F32)
    u, ru = c.sb("u", [128, D], BF16)
    wts = [c.sb("wt%d" % i, [128, 16, 512], BF16) for i in range(2)]
    xss = [c.sb("xs%d" % i, [128, 4, 128], F32) for i in range(2)]
    tmp = [c.sb("tmp%d" % i, [128, 4, 64], F32) for i in range(4)]
    rbs = [c.sb("rb%d" % i, [128, 4, 128], BF16) for i in range(2)]
    tss = [c.sb("ts%d" % i, [128, 4, 128], BF16) for i in range(2)]
    gss = [c.sb("gs%d" % i, [128, 48], F32) for i in range(2)]
    pT, rpT = c.ps("pT", [128, 4, 128], BF16)
    pys = [c.ps("py%d" % i, [128, 512], F32) for i in range(2)]

    c.load("sp", gt[:], g.partition_broadcast(128), rgt)
    c.load("sp", idt[:], ident, rid)
    c.load("sp", gbt[:], gb.partition_broadcast(128), rgb)
    for i in range(4):
        c.load("sp", rt[:, i, :, :], ropet[i].rearrange("(t p) d -> p t d", p=128), rrt)

    for tt in range(ntile):
        xt, rx = xts[tt % 2]
        c.load("sp", xt[:], x[tt * 128:(tt + 1) * 128, :], rx)
        rmsnorm_tile(c, xt, rx, gt, rgt, u, ru, junk, rj, ss, rss)
        transpose_to(c, u, ru, 16, lambda k0, n, tt=tt: uT[:, k0:k0 + n, tt * 128:(tt + 1) * 128], ruT, pT, rpT, idt, rid)

    nblk = 11
    it = 0
    for cb in range(nblk):
        ncol = min(512, NSA_W - cb * 512)
        wt, rw = wts[cb % 2]
        c.load("pool", wt[:, :, 0:ncol], w[:, cb * 512:cb * 512 + ncol].rearrange("(kc p) n -> p kc n", p=128), rw)
        for tt in range(ntile):
            py, rpy = pys[it % 2]
            xs, rxs = xss[it % 2]
            rb, rrb = rbs[it % 2]
            ts_, rts = tss[it % 2]
            gs, rgs = gss[it % 2]
            it += 1
            for kc in range(16):
                S.op("pe", lambda E, kc=kc, py=py, wt=wt, tt=tt, ncol=ncol: E.matmul(
                    py[:, 0:ncol], lhsT=uT[:, kc, tt * 128:(tt + 1) * 128], rhs=wt[:, kc, 0:ncol],
                    start=(kc == 0), stop=(kc == 15)), reads=[ruT, rw], writes=[rpy])
            pyv = py[:].rearrange("p (h d) -> p h d", h=4)
            if cb in (0, 1, 2, 3, 6, 8):
                ci = 0 if cb < 4 else 2
                cos = rt[:, ci, tt, :].unsqueeze(1).to_broadcast([128, 4, 64])
                sin = rt[:, ci + 1, tt, :].unsqueeze(1).to_broadcast([128, 4, 64])
                S.op("act", lambda E, xs=xs, pyv=pyv: E.activation(out=xs[:], in_=pyv, func=AF.Copy), reads=[rpy], writes=[rxs])
                x1 = xs[:, :, 0:64]
                x2 = xs[:, :, 64:128]
                (t1, r1), (t2, r2), (t3, r3), (t4, r4) = tmp
                S.op("dve", lambda E, x1=x1, cos=cos, t1=t1: E.tensor_tensor(out=t1[:], in0=x1, in1=cos, op=ALU.mult), reads=[rxs, rrt], writes=[r1])
                S.op("dve", lambda E, x2=x2, sin=sin, t2=t2: E.tensor_tensor(out=t2[:], in0=x2, in1=sin, op=ALU.mult), reads=[rxs, rrt], writes=[r2])
                S.op("dve", lambda E, rb=rb, t1=t1, t2=t2: E.tensor_tensor(out=rb[:, :, 0:64], in0=t1[:], in1=t2[:], op=ALU.subtract), reads=[r1, r2], writes=[rrb])
                S.op("dve", lambda E, x2=x2, cos=cos, t3=t3: E.tensor_tensor(out=t3[:], in0=x2, in1=cos, op=ALU.mult), reads=[rxs, rrt], writes=[r3])
                S.op("dve", lambda E, x1=x1, sin=sin, t4=t4: E.tensor_tensor(out=t4[:], in0=x1, in1=sin, op=ALU.mult), reads=[rxs, rrt], writes=[r4])
                S.op("dve", lambda E, rb=rb, t3=t3, t4=t4: E.tensor_tensor(out=rb[:, :, 64:128], in0=t3[:], in1=t4[:], op=ALU.add), reads=[r3, r4], writes=[rrb])
            elif cb == 10:
                S.op("dve", lambda E, gs=gs, py=py: E.tensor_tensor(out=gs[:], in0=py[:, 0:48], in1=gbt[:], op=ALU.add), reads=[rpy, rgb], writes=[rgs])
                S.op("act", lambda E, gs=gs: E.activation(out=gs[:], in_=gs[:], func=AF.Sigmoid), reads=[rgs], writes=[rgs])
                c.store("sp", gates[tt * 128:(tt + 1) * 128, :], gs[:], rgs)
                continue
            else:
                S.op("act", lambda E, rb=rb, pyv=pyv: E.activation(out=rb[:], in_=pyv, func=AF.Copy), reads=[rpy], writes=[rrb])
            if cb in (7, 9):
                c.store("sp", vtok[0 if cb == 7 else 1, tt * 128:(tt + 1) * 128, :], rb[:].rearrange("p h d -> p (h d)"), rrb)
            else:
                bi = {0: 0, 1: 1, 2: 2, 3: 3, 4: 4, 5: 5, 6: 6, 8: 7}[cb]
                rbf = rb[:].rearrange("p h d -> p (h d)")
                transpose_to(c, rbf, rrb, 4, lambda k0, n, ts_=ts_: ts_[:, k0:k0 + n, :], rts, pT, rpT, idt, rid)
                c.store("sp", fT[bi, :, :, tt * 128:(tt + 1) * 128].rearrange("h d t -> d h t"), ts_[:], rts)
    return c.finish()


def rope_tables(pos, scale=1.0):
    half = 64
    inv = (10000.0 ** (-np.arange(half, dtype=np.float32) / half)).astype(np.float32)
    ang = pos.astype(np.float32)[:, None] * inv[None, :]
    return (np.cos(ang) * scale).astype(np.float32), (np.sin(ang) * scale).astype(np.float32)


def build_k2():
    c = Ctx()
    S = c.S
    XT = 2048 + 16
    xT_d = c.din("xT", [2, 4, 128, XT], BF16)
    w1_d = c.din("w1", [2, 4096, 256], F32)
    w2_d = c.din("w2", [2, 256, 128], F32)
    peT_d = c.din("peT", [2, 128, 32], F32)
    ropec = c.din("ropec", [2, 128, 64], F32)
    ident = c.din("ident", [128, 128], BF16)
    kcT_o = c.dout("kcT", [4, 128, 128], BF16)
    vc_o = c.dout("vc", [128, 4, 128], BF16)

    xT, rxT = c.sb("xTs", [128, 2, 4, XT], BF16)
    w1, rw1 = c.sb("w1s", [128, 2, 32, 256], BF16)
    w2, rw2 = c.sb("w2s", [128, 2, 2, 128], BF16)
    pef, rpef = c.sb("pef", [128, 2, 32], F32)
    peb, rpeb = c.sb("peb", [128, 2, 32], BF16)
    rc, rrc = c.sb("rc", [128, 2, 64], F32)
    idt, rid = c.sb("idt", [128, 128], BF16)
    bias, rbias = c.sb("bias", [128, 4], F32)
    xa, rxa = c.sb("xa", [128, 128], F32)
    t1, r1 = c.sb("t1", [128, 128], F32)
    t2, r2 = c.sb("t2", [128, 128], F32)
    h1T, rh1 = c.sb("h1T", [128, 2, 128], BF16)
    xs, rxs = c.sb("xs", [128, 128], F32)
    rb, rrb = c.sb("rb", [128, 128], BF16)
    kst, rkst = c.sb("kst", [128, 4, 128], BF16)
    vst, rvst = c.sb("vst", [128, 4, 128], BF16)
    pb, rpb = c.ps("pb", [128, 4], F32)
    ph, rph = c.ps("ph", [128, 128], F32)
    pk, rpk = c.ps("pk", [128, 128], F32)
    pT, rpT = c.ps("pT", [128, 4, 128], BF16)

    c.load("sp", xT[:], xT_d.rearrange("a g d t -> d a g t"), rxT)
    for kv in range(2):
        c.load("pool", w1[:, kv, :, :], w1_d[kv].rearrange("(p d) j -> d p j", d=128), rw1)
        c.load("pool", w2[:, kv, :, :], w2_d[kv].rearrange("(jh j) d -> j jh d", j=128), rw2)
    c.load("sp", pef[:], peT_d.rearrange("a d p -> d a p"), rpef)
    c.load("sp", rc[:], ropec.rearrange("a c d -> c a d"), rrc)
    c.load("sp", idt[:], ident, rid)
    S.op("dve", lambda E: E.tensor_copy(out=peb[:], in_=pef[:]), reads=[rpef], writes=[rpeb])
    for kv in range(2):
        for jh in range(2):
            for p in range(32):
                S.op("pe", lambda E, kv=kv, jh=jh, p=p: E.matmul(
                    pb[:, kv * 2 + jh:kv * 2 + jh + 1], lhsT=w1[:, kv, p, jh * 128:(jh + 1) * 128], rhs=peb[:, kv, p:p + 1],
                    start=(p == 0), stop=(p == 31)), reads=[rw1, rpeb], writes=[rpb])
    S.op("dve", lambda E: E.tensor_copy(out=bias[:], in_=pb[:]), reads=[rpb], writes=[rbias])
    for kv in range(2):
        for g in range(4):
            for jh in range(2):
                for p in range(32):
                    S.op("pe", lambda E, kv=kv, jh=jh, p=p, g=g: E.matmul(
                        ph[:], lhsT=w1[:, kv, p, jh * 128:(jh + 1) * 128], rhs=xT[:, kv, g, p:p + 2033:16],
                        start=(p == 0), stop=(p == 31)), reads=[rw1, rxT], writes=[rph])
                S.op("act", lambda E, kv=kv, jh=jh: E.activation(out=xa[:], in_=ph[:], func=AF.Identity,
                                                                 bias=bias[:, kv * 2 + jh:kv * 2 + jh + 1]), reads=[rph, rbias], writes=[rxa])
                S.op("dve", lambda E: E.tensor_tensor(out=t1[:], in0=xa[:], in1=xa[:], op=ALU.mult), reads=[rxa], writes=[r1])
                S.op("dve", lambda E: E.tensor_scalar(out=t1[:], in0=t1[:], scalar1=0.044715, scalar2=1.0, op0=ALU.mult, op1=ALU.add), reads=[r1], writes=[r1])
                S.op("dve", lambda E: E.tensor_tensor(out=t1[:], in0=t1[:], in1=xa[:], op=ALU.mult), reads=[r1, rxa], writes=[r1])
                S.op("act", lambda E: E.activation(out=t2[:], in_=t1[:], func=AF.Sigmoid, scale=1.5957691216057308), reads=[r1], writes=[r2])
                S.op("dve", lambda E, jh=jh: E.tensor_tensor(out=h1T[:, jh, :], in0=xa[:], in1=t2[:], op=ALU.mult), reads=[rxa, r2], writes=[rh1])
            for jh in range(2):
                S.op("pe", lambda E, kv=kv, jh=jh: E.matmul(pk[:], lhsT=h1T[:, jh, :], rhs=w2[:, kv, jh, :], start=(jh == 0), stop=(jh == 1)),
                     reads=[rh1, rw2], writes=[rpk])
            if kv == 0:
                S.op("act", lambda E: E.activation(out=xs[:], in_=pk[:], func=AF.Copy), reads=[rpk], writes=[rxs])
                cos = rc[:, 0, :]
                sin = rc[:, 1, :]
                S.op("dve", lambda E: E.tensor_tensor(out=t1[:, 0:64], in0=xs[:, 0:64], in1=cos, op=ALU.mult), reads=[rxs, rrc], writes=[r1])
                S.op("dve", lambda E: E.tensor_tensor(out=t1[:, 64:128], in0=xs[:, 64:128], in1=sin, op=ALU.mult), reads=[rxs, rrc], writes=[r1])
                S.op("dve", lambda E: E.tensor_tensor(out=rb[:, 0:64], in0=t1[:, 0:64], in1=t1[:, 64:128], op=ALU.subtract), reads=[r1], writes=[rrb])
                S.op("dve", lambda E: E.tensor_tensor(out=t2[:, 0:64], in0=xs[:, 64:128], in1=cos, op=ALU.mult), reads=[rxs, rrc], writes=[r2])
                S.op("dve", lambda E: E.tensor_tensor(out=t2[:, 64:128], in0=xs[:, 0:64], in1=sin, op=ALU.mult), reads=[rxs, rrc], writes=[r2])
                S.op("dve", lambda E: E.tensor_tensor(out=rb[:, 64:128], in0=t2[:, 0:64], in1=t2[:, 64:128], op=ALU.add), reads=[r2], writes=[rrb])
                S.op("pe", lambda E: E.transpose(out=pT[:, 0, :], in_=rb[:], identity=idt[:]), reads=[rrb, rid], writes=[rpT])
                S.op("dve", lambda E, g=g: E.tensor_copy(out=kst[:, g, :], in_=pT[:, 0, :]), reads=[rpT], writes=[rkst])
            else:
                S.op("act", lambda E, g=g: E.activation(out=vst[:, g, :], in_=pk[:], func=AF.Copy), reads=[rpk], writes=[rvst])
    c.store("sp", kcT_o.rearrange("g d c -> d g c"), kst[:], rkst)
    c.store("sp", vc_o, vst[:], rvst)
    return c.finish()


POOLENG = 'dve'


def build_k3(nround=16, stage=99):
    c = Ctx()

    def chk(n):
        if stage == n:
            c.S.off = True
    S = c.S
    NKT = 8 * nround
    NCK = (nround - 1) // 2 + 1
    qT_d = c.din("qT", [nround, 16, 128, 128], BF16)
    ksT_d = c.din("ksT", [4, 128, SEQ], BF16)
    vs_d = c.din("vs", [SEQ, 4, 128], BF16)
    kcT_d = c.din("kcT", [4, 128, 1024], BF16)
    vc_d = c.din("vc", [1024, 4, 128], BF16)
    kwT_d = c.din("kwT", [nround, 4, 128, 640], BF16)
    vw_d = c.din("vw", [nround, 640, 4, 128], BF16)
    gates_d = c.din("gates", [nround, 128, 48], F32)
    cmask_d = c.din("cmask", [nround, 2, 128, 128], BF16)
    dmask_d = c.din("dmask", [8, 128, 128], BF16)
    wmask_d = c.din("wmask", [nround, 5, 128, 128], BF16)
    fadd_d = c.din("fadd", [nround, 128, 256], F32)
    E_d = c.din("Econst", [64, 128, 128], BF16)
    W_d = c.din("Wones", [8, 128, 257], BF16)
    ident = c.din("ident", [128, 128], BF16)
    o_d = c.dout("o", [nround, 128, 2048], BF16)

    ks_sb, rks = c.sb("ks_sb", [128, NKT * 128], BF16)
    vs_sb, rvs = c.sb("vs_sb", [128, NKT, 129], BF16)
    kc_sb, rkc = c.sb("kc_sb", [128, 4, 1024], BF16)
    R_sb, rR = c.sb("R_sb", [128, 4, 8, 385], BF16)
    E_sb, rE = c.sb("E_sb", [128, 64, 128], BF16)
    idt, rid = c.sb("idt", [128, 128], BF16)
    dm_sb, rdm = c.sb("dm_sb", [128, 8, 128], BF16)
    q_sbs = [c.sb("q_sb%d" % i, [128, 4, 128], BF16) for i in range(2)]
    pTs = [c.sb("pTs%d" % i, [128, 4, 128], BF16) for i in range(3)]
    kw_sbs = [c.sb("kw_sb%d" % i, [128, 640], BF16) for i in range(2)]
    vw_sbs = [c.sb("vw_sb%d" % i, [128, 5, 129], BF16) for i in range(2)]
    cm_sbs = [c.sb("cm_sb%d" % i, [128, 2, 128], BF16) for i in range(2)]
    wm_sbs = [c.sb("wm_sb%d" % i, [128, 5, 128], BF16) for i in range(2)]
    fa_sbs = [c.sb("fa_sb%d" % i, [128, 256], F32) for i in range(2)]
    gt_sbs = [c.sb("gt_sb%d" % i, [128, 48], F32) for i in range(2)]
    cs, rcs = c.sb("cs", [128, 4, 385], F32)
    os_, ros = c.sb("os", [128, 4, 129], F32)
    ow, row = c.sb("ow", [128, 4, 129], F32)
    den, rden = c.sb("den", [128, 3, 4], F32)
    coef, rcoef = c.sb("coef", [128, 3, 4], F32)
    sblk, rsblk = c.sb("sblk", [128, 256], F32)
    wk, rwk = c.sb("wk", [128, 256], F32)
    m8, rm8 = c.sb("m8", [128, 8], F32)
    thr, rthr = c.sb("thr", [128, 1], F32)
    sel, rsel = c.sb("sel", [128, 256], BF16)
    selT, rselT = c.sb("selT", [128, 2, 128], BF16)
    selT4, rselT4 = c.sb("selT4", [128, 2, 4, 128], BF16)
    oacc, roacc = c.sb("oacc", [128, 4, 128], F32)
    otmp, rotmp = c.sb("otmp", [128, 4, 128], F32)
    o_sbs = [c.sb("o_sb%d" % i, [128, 4, 128], BF16) for i in range(2)]

    SB = [c.ps("SB%d" % i, [128, 4, 128], F32) for i in range(2)]
    MB, _ = c.ps("MB", [128, 4, 128], F32)
    rMB = [Res() for _ in range(4)]
    TB, rTB = c.ps("TB", [128, 8, 128], BF16)
    ACC = [c.ps("ACC%d" % i, [128, 512], F32) for i in range(4)]

    c.load("sp", kc_sb[:], kcT_d.rearrange("g d c -> d g c"), rkc)
    for g in range(4):
        c.load("sp", R_sb[:, g, :, 0:128], vc_d[:, g, :].rearrange("(ch p) d -> p ch d", p=128), rR)
        c.load("sp", R_sb[:, g, :, 128:385], W_d.rearrange("ch p w -> p ch w"), rR)
    c.load("sp", E_sb[:], E_d.rearrange("m b k -> b m k"), rE)
    c.load("sp", idt[:], ident, rid)
    c.load("sp", dm_sb[:], dmask_d.rearrange("j k q -> k j q"), rdm)
    S.op("dve", lambda E: E.memset(vs_sb[:, :, 128:129], 1.0), writes=[rvs])
    for i in range(2):
        S.op("dve", lambda E, i=i: E.memset(vw_sbs[i][0][:, :, 128:129], 1.0), writes=[vw_sbs[i][1]])

    state = {"s": 0, "p": 0, "m": 0}

    def branch(n, q_sb, rq, k_fn, rk, v_fn, rv, ncol, pre_fn, post_fn):
        def issue_s(kt):
            sb, rsb = SB[state["s"] % 2]
            state["s"] += 1
            S.op("pe", lambda E, kt=kt, sb=sb: E.matmul(sb[:].rearrange("p h q -> p (h q)"), lhsT=k_fn(kt),
                                                       rhs=q_sb[:].rearrange("p h q -> p (h q)"), start=True, stop=(pre_fn is None)),
                 reads=[rk, rq], writes=[rsb])
            if pre_fn:
                pre_fn(kt, sb, rsb)
            return sb, rsb, None
        nxt = issue_s(0)
        for kt in range(n):
            sb, rsb, extra = nxt
            if kt + 1 < n:
                nxt = issue_s(kt + 1)
            pT, rpT = pTs[state["p"] % 3]
            state["p"] += 1
            S.op("act", lambda E, pT=pT, sb=sb: E.activation(out=pT[:], in_=sb[:], func=AF.Exp), reads=[rsb], writes=[rpT])
            post_fn(kt, pT, rpT, extra)
            for h in range(4):
                acc, racc = ACC[h]
                S.op("pe", lambda E, h=h, kt=kt, pT=pT, acc=acc: E.matmul(acc[:, 0:ncol], lhsT=pT[:, h, :], rhs=v_fn(kt),
                                                                        start=(kt == 0), stop=(kt == n - 1)),
                     reads=[rpT, rv], writes=[racc])

    def bc(ap2d):
        return ap2d.unsqueeze(1).to_broadcast([128, 4, 128])

    chk(0)
    it = 0
    for g in range(4):
        c.load("sp", ks_sb[:], ksT_d[g, :, 0:NKT * 128], rks)
        for t0 in range(0, NKT, 32):
            t1 = min(NKT, t0 + 32)
            c.load("sp", vs_sb[:, t0:t1, 0:128], vs_d[t0 * 128:t1 * 128, g, :].rearrange("(t p) d -> p t d", p=128), rvs)
        for r in range(nround):
            b = it % 2
            it += 1
            q_sb, rq = q_sbs[b]
            kw_sb, rkw = kw_sbs[b]
            vw_sb, rvw = vw_sbs[b]
            cm_sb, rcm = cm_sbs[b]
            wm_sb, rwm = wm_sbs[b]
            fa_sb, rfa = fa_sbs[b]
            gt_sb, rgt = gt_sbs[b]
            o_sb, ro = o_sbs[b]
            c.load("sp", q_sb[:], qT_d[r, 4 * g:4 * g + 4].rearrange("h d q -> d h q"), rq)
            c.load("sp", cm_sb[:], cmask_d[r].rearrange("a k q -> k a q"), rcm)
            c.load("sp", fa_sb[:], fadd_d[r], rfa)
            c.load("sp", gt_sb[:], gates_d[r], rgt)
            c.load("sp", kw_sb[:], kwT_d[r, g], rkw)
            c.load("sp", vw_sb[:, :, 0:128], vw_d[r, :, g, :].rearrange("(j p) d -> p j d", p=128), rvw)
            c.load("sp", wm_sb[:], wmask_d[r].rearrange("j k q -> k j q"), rwm)
            nck = r // 2 + 1
            nsc = r // 8 + 1
            nkt = 8 * (r + 1)
            chk(1)

            def cmp_post(kt, pT, rpT, extra, nck=nck, cm_sb=cm_sb, rcm=rcm):
                if kt == nck - 1:
                    S.op("dve", lambda E: E.tensor_tensor(out=pT[:], in0=pT[:], in1=bc(cm_sb[:, 1, :]), op=ALU.mult), reads=[rpT, rcm], writes=[rpT])
                elif kt == nck - 2:
                    S.op("dve", lambda E: E.tensor_tensor(out=pT[:], in0=pT[:], in1=bc(cm_sb[:, 0, :]), op=ALU.mult), reads=[rpT, rcm], writes=[rpT])
            branch(nck, q_sb, rq, lambda kt, g=g: kc_sb[:, g, kt * 128:(kt + 1) * 128], rkc,
                   lambda kt, g=g: R_sb[:, g, kt, :], rR, 385, None, cmp_post)
            for h in range(4):
                acc, racc = ACC[h]
                if h % 2 == 0:
                    S.op("act", lambda E, h=h, acc=acc: E.activation(out=cs[:, h, :], in_=acc[:, 0:385], func=AF.Copy), reads=[racc], writes=[rcs])
                else:
                    S.op("dve", lambda E, h=h, acc=acc: E.tensor_copy(out=cs[:, h, :], in_=acc[:, 0:385]), reads=[racc], writes=[rcs])
            chk(2)
            S.op("dve", lambda E: E.tensor_scalar(out=den[:, 0, :], in0=cs[:, :, 128], scalar1=1e-30, scalar2=None, op0=ALU.max), reads=[rcs], writes=[rden])
            S.op("dve", lambda E: E.reciprocal(out=den[:, 0, :], in_=den[:, 0, :]), reads=[rden], writes=[rden])
            S.op("dve", lambda E: E.tensor_scalar(out=sblk[:], in0=cs[:, 0, 129:385], scalar1=den[:, 0, 0:1], scalar2=None, op0=ALU.mult), reads=[rcs, rden], writes=[rsblk])
            for h in range(1, 4):
                S.op("dve", lambda E, h=h: E.scalar_tensor_tensor(out=sblk[:], in0=cs[:, h, 129:385], scalar=den[:, 0, h:h + 1], in1=sblk[:],
                                                                  op0=ALU.mult, op1=ALU.add), reads=[rcs, rden, rsblk], writes=[rsblk])
            S.op("dve", lambda E, fa_sb=fa_sb: E.tensor_tensor(out=sblk[:], in0=sblk[:], in1=fa_sb[:], op=ALU.add), reads=[rsblk, rfa], writes=[rsblk])
            chk(3)
            S.op("dve", lambda E: E.max(out=m8[:], in_=sblk[:]), reads=[rsblk], writes=[rm8])
            S.op("dve", lambda E: E.match_replace(out=wk[:], in_to_replace=m8[:], in_values=sblk[:], imm_value=-3.0e38), reads=[rm8, rsblk], writes=[rwk])
            S.op("dve", lambda E: E.max(out=m8[:], in_=wk[:]), reads=[rwk], writes=[rm8])
            S.op("dve", lambda E: E.tensor_reduce(out=thr[:], in_=m8[:], axis=AX.X, op=ALU.min), reads=[rm8], writes=[rthr])
            S.op("dve", lambda E: E.tensor_scalar(out=sel[:], in0=sblk[:], scalar1=thr[:, 0:1], scalar2=-30000.0, op0=ALU.is_lt, op1=ALU.mult), reads=[rsblk, rthr], writes=[rsel])
            for sc in range(nsc):
                S.op("pe", lambda E, sc=sc: E.transpose(out=TB[:, sc, :], in_=sel[:, sc * 128:(sc + 1) * 128], identity=idt[:]), reads=[rsel, rid], writes=[rTB])
            S.op("dve", lambda E, nsc=nsc: E.tensor_copy(out=selT[:, 0:nsc, :], in_=TB[:, 0:nsc, :]), reads=[rTB], writes=[rselT])
            for sc in range(nsc):
                S.op("dve", lambda E, sc=sc: E.tensor_copy(out=selT4[:, sc, :, :], in_=bc(selT[:, sc, :])), reads=[rselT], writes=[rselT4])

            chk(4)
            def sel_pre(kt, sb, rsb):
                S.op("pe", lambda E, kt=kt, sb=sb: E.matmul(sb[:].rearrange("p h q -> p (h q)"), lhsT=E_sb[:, kt % 64, :],
                                                            rhs=selT4[:, kt // 64, :, :].rearrange("p h q -> p (h q)"), start=False, stop=True),
                     reads=[rE, rselT4], writes=[rsb])

            def sel_post(kt, pT, rpT, slot, nkt=nkt):
                if kt >= nkt - 8:
                    j = kt - (nkt - 8)
                    S.op("dve", lambda E: E.tensor_tensor(out=pT[:], in0=pT[:], in1=bc(dm_sb[:, j, :]), op=ALU.mult), reads=[rpT, rdm], writes=[rpT])
            branch(nkt, q_sb, rq, lambda kt: ks_sb[:, kt * 128:(kt + 1) * 128], rks,
                   lambda kt: vs_sb[:, kt, :], rvs, 129, sel_pre, sel_post)
            for h in range(4):
                acc, racc = ACC[h]
                if h % 2 == 0:
                    S.op("act", lambda E, h=h, acc=acc: E.activation(out=os_[:, h, :], in_=acc[:, 0:129], func=AF.Copy), reads=[racc], writes=[ros])
                else:
                    S.op("dve", lambda E, h=h, acc=acc: E.tensor_copy(out=os_[:, h, :], in_=acc[:, 0:129]), reads=[racc], writes=[ros])

            chk(5)
            def win_post(kt, pT, rpT, extra, wm_sb=wm_sb, rwm=rwm):
                S.op(POOLENG, lambda E: E.tensor_tensor(out=pT[:], in0=pT[:], in1=bc(wm_sb[:, kt, :]), op=ALU.mult), reads=[rpT, rwm], writes=[rpT])
            branch(5, q_sb, rq, lambda kt, kw_sb=kw_sb: kw_sb[:, kt * 128:(kt + 1) * 128], rkw,
                   lambda kt, vw_sb=vw_sb: vw_sb[:, kt, :], rvw, 129, None, win_post)
            for h in range(4):
                acc, racc = ACC[h]
                if h % 2 == 0:
                    S.op("act", lambda E, h=h, acc=acc: E.activation(out=ow[:, h, :], in_=acc[:, 0:129], func=AF.Copy), reads=[racc], writes=[row])
                else:
                    S.op("dve", lambda E, h=h, acc=acc: E.tensor_copy(out=ow[:, h, :], in_=acc[:, 0:129]), reads=[racc], writes=[row])

            chk(6)
            S.op("dve", lambda E: E.tensor_scalar(out=den[:, 1, :], in0=os_[:, :, 128], scalar1=1e-30, scalar2=None, op0=ALU.max), reads=[ros], writes=[rden])
            S.op("dve", lambda E: E.tensor_scalar(out=den[:, 2, :], in0=ow[:, :, 128], scalar1=1e-30, scalar2=None, op0=ALU.max), reads=[row], writes=[rden])
            S.op("dve", lambda E: E.reciprocal(out=den[:, 1:3, :], in_=den[:, 1:3, :]), reads=[rden], writes=[rden])
            gv = gt_sb[:].rearrange("p (b g h) -> p b g h", b=3, g=4)[:, :, g, :]
            S.op("dve", lambda E, gv=gv: E.tensor_tensor(out=coef[:], in0=den[:], in1=gv, op=ALU.mult), reads=[rden, rgt], writes=[rcoef])

            def cb(i):
                return coef[:, i, :].unsqueeze(2).to_broadcast([128, 4, 128])
            S.op("dve", lambda E: E.tensor_tensor(out=oacc[:], in0=cs[:, :, 0:128], in1=cb(0), op=ALU.mult), reads=[rcs, rcoef], writes=[roacc])
            S.op(POOLENG, lambda E: E.tensor_tensor(out=otmp[:], in0=os_[:, :, 0:128], in1=cb(1), op=ALU.mult), reads=[ros, rcoef], writes=[rotmp])
            S.op("dve", lambda E: E.tensor_tensor(out=oacc[:], in0=oacc[:], in1=otmp[:], op=ALU.add), reads=[roacc, rotmp], writes=[roacc])
            S.op(POOLENG, lambda E: E.tensor_tensor(out=otmp[:], in0=ow[:, :, 0:128], in1=cb(2), op=ALU.mult), reads=[row, rcoef], writes=[rotmp])
            S.op("dve", lambda E, o_sb=o_sb: E.tensor_tensor(out=o_sb[:], in0=oacc[:], in1=otmp[:], op=ALU.add), reads=[roacc, rotmp], writes=[ro])
            c.store("sp", o_d[r, :, g * 512:(g + 1) * 512], o_sb[:].rearrange("p h d -> p (h d)"), ro)
    return c.finish()


def k3_consts():
    E = np.zeros((64, 128, 128), np.float32)
    key = np.arange(128)
    for m in range(64):
        E[m, 2 * m + key // 64, key] = 1.0
    W = np.zeros((1024, 257), np.float32)
    W[:, 0] = 1.0
    for j in range(256):
        for cc, wv in ((4 * j - 1, 1.0), (4 * j, 2.0), (4 * j + 1, 2.0), (4 * j + 2, 2.0), (4 * j + 3, 1.0)):
            if 0 <= cc < 1023:
                W[cc, 1 + j] = wv
    return E.astype(NPBF), W.reshape(8, 128, 257).astype(NPBF)


def k3_core_masks(ci, nround):
    ql = np.arange(128)
    kl = np.arange(128)
    cmask = np.zeros((nround, 2, 128, 128), np.float32)
    wmask = np.zeros((nround, 5, 128, 128), np.float32)
    fadd = np.zeros((nround, 128, 256), np.float32)
    dmask = np.zeros((8, 128, 128), np.float32)
    tri = (kl[:, None] <= ql[None, :]).astype(np.float32)
    for j in range(8):
        dmask[j] = 1.0 if j < ci else (tri if j == ci else 0.0)
    blk = np.arange(256)
    for r in range(nround):
        qb = 8 * r + ci
        qpos = 128 * qb + ql
        chl = qb // 16
        for a, ch in ((1, chl), (0, chl - 1)):
            if ch < 0:
                continue
            cpos = 16 * (128 * ch + kl) + 31
            cmask[r, a] = (cpos[:, None] <= qpos[None, :]) & ((128 * ch + kl) < 1023)[:, None]
        for j in range(5):
            kt = qb - 4 + j
            if kt < 0:
                continue
            kpos = 128 * kt + kl
            diff = qpos[None, :] - kpos[:, None]
            wmask[r, j] = (diff >= 0) & (diff < 512)
        cur = qpos // 64
        valid = blk[None, :] * 64 <= qpos[:, None]
        fa = np.where(valid, 0.0, -1e30).astype(np.float32)
        rows = np.arange(128)
        fa[:, 0] = 1e9
        m1 = cur - 1 >= 1
        fa[rows[m1], (cur - 1)[m1]] = 4e9
        fa[rows, cur] = np.where(cur == 0, 1e9, 2e9)
        fadd[r] = fa
    return cmask.astype(NPBF), dmask.astype(NPBF), wmask.astype(NPBF), fadd


def k3_inputs(ci, nround, qT_full, ksT_full, vs_full, kcT_full, vc_full, kwT_full, vw_full, gates_full, consts):
    E, W, ident = consts
    qT = np.zeros((nround, 16, 128, 128), NPBF)
    kwT = np.zeros((nround, 4, 128, 640), NPBF)
    vw = np.zeros((nround, 640, 4, 128), NPBF)
    gates = np.zeros((nround, 128, 48), np.float32)
    for r in range(nround):
        qb = 8 * r + ci
        qT[r] = qT_full[:, :, 128 * qb:128 * qb + 128]
        lo = 128 * (qb - 4)
        hi = 128 * (qb + 1)
        s0 = max(lo, 0)
        kwT[r, :, :, s0 - lo:] = kwT_full[:, :, s0:hi]
        vw[r, s0 - lo:] = vw_full[s0:hi]
        gates[r] = gates_full[128 * qb:128 * qb + 128]
    cmask, dmask, wmask, fadd = k3_core_masks(ci, nround)
    return {"qT": qT, "ksT": ksT_full, "vs": vs_full, "kcT": kcT_full, "vc": vc_full, "kwT": kwT, "vw": vw,
            "gates": gates, "cmask": cmask, "dmask": dmask, "wmask": wmask, "fadd": fadd,
            "Econst": E, "Wones": W, "ident": ident}


def build_kb(mode, ntile=16, c=None):
    c = c or Ctx()
    S = c.S
    T = ntile * 128
    xin = c.din("xin", [T, D], F32)
    ident = c.din("ident", [128, 128], BF16)
    mem = c.din("mem", [256, D], F32)
    g_kv = c.din("g_kv", [D], F32)
    g_q = c.din("g_q", [D], F32)
    wq_d = c.din("wq", [D, 512], F32)
    wk_d = c.din("wk", [D, 512], F32)
    wv_d = c.din("wv", [D, 512], F32)
    wo_d = c.din("wo", [512, D], F32)
    hout = c.dout("hout", [T, D], F32)
    if mode == "nsa":
        o_d = c.din("o", [T, D], BF16)
        wout_d = c.din("w_out", [D, D], F32)
        wbig, rwbig = c.sb("wbig", [128, 16, D], BF16)
        c.load("pool", wbig[:, 0:8, :], wout_d[0:1024, :].rearrange("(kc p) n -> p kc n", p=128), rwbig)
        c.load("pool", wbig[:, 8:16, :], wout_d[1024:2048, :].rearrange("(kc p) n -> p kc n", p=128), rwbig)
        o_sbs = [c.sb("o_in%d" % i, [128, D], BF16) for i in range(2)]
    else:
        halo = c.din("halo", [128, D], F32)
        g_mix = c.din("g_mix", [D], F32)
        Bm_d = c.din("Bm", [128, 3, 4, 128], BF16)
        wp_d = c.din("wp", [4, 512, 512], F32)
        bp_d = c.din("bp", [D], F32)
        sc_d = c.din("sc", [D], F32)
        wp, rwp = c.sb("wp_sb", [128, 4, 4, 512], BF16)
        for gi in range(4):
            c.load("pool", wp[:, gi, :, :], wp_d[gi].rearrange("(cc p) e -> p cc e", p=128), rwp)
        Bm, rBm = c.sb("Bm_sb", [128, 3, 4, 128], BF16)
        c.load("sp", Bm[:], Bm_d, rBm)
        gm, rgm = c.sb("gm", [128, D], F32)
        bt, rbt = c.sb("bt", [128, D], F32)
        sct, rsct = c.sb("sct", [128, D], F32)
        c.load("sp", gm[:], g_mix.partition_broadcast(128), rgm)
        c.load("sp", bt[:], bp_d.partition_broadcast(128), rbt)
        c.load("sp", sct[:], sc_d.partition_broadcast(128), rsct)
        us = [c.sb("upool%d" % i, [128, D], BF16) for i in range(2)]
        dT, rdT = c.sb("dT", [128, 16, 128], BF16)
        zt, rzt = c.sb("zt", [128, 512], F32)

    idt, rid = c.sb("idt", [128, 128], BF16)
    c.load("sp", idt[:], ident, rid)
    wq, rwq = c.sb("wq_sb", [128, 16, 512], BF16)
    wkv, rwkv = c.sb("wkv_sb", [128, 16, 512], BF16)
    wo, rwo = c.sb("wo_sb", [128, 4, D], BF16)
    c.load("pool", wq[:], wq_d.rearrange("(kc p) n -> p kc n", p=128), rwq)
    c.load("pool", wkv[:], wk_d.rearrange("(kc p) n -> p kc n", p=128), rwkv)
    c.load("pool", wo[:], wo_d.rearrange("(kc p) n -> p kc n", p=128), rwo)
    gA, rgA = c.sb("gA", [128, D], F32)
    c.load("sp", gA[:], g_kv.partition_broadcast(128), rgA)
    xt, rx = c.sb("xt", [128, D], F32)
    h1, rh1 = c.sb("h1", [128, D], F32)
    junk, rj = c.sb("junk", [128, D], BF16)
    ss, rss = c.sb("ss", [128, 2], F32)
    u, ru = c.sb("u", [128, D], BF16)
    aT, raT = c.sb("aT", [128, 16, 128], BF16)
    memnT, rmT = c.sb("memnT", [128, 16, 256], BF16)
    kmT, rkmT = c.sb("kmT", [128, 4, 256], BF16)
    vm, rvm = c.sb("vm", [128, 2, 4, 129], BF16)
    kq, rkq = c.sb("kq", [128, 512], BF16)
    qmT, rqmT = c.sb("qmT", [128, 4, 128], BF16)
    pm, rpm = c.sb("pm", [128, 2, 4, 128], BF16)
    om32, rom32 = c.sb("om32", [128, 4, 129], F32)
    rdn, rrdn = c.sb("rdn", [128, 4], F32)
    om, rom = c.sb("om", [128, 4, 128], BF16)
    omT, romT = c.sb("omT", [128, 4, 128], BF16)
    ho, rho = c.sb("ho", [128, D], F32)

    A = [c.ps("A%d" % i, [128, 512], F32) for i in range(4)]
    TB, rTB = c.ps("TB", [128, 4, 128], BF16)
    QB, rQB = c.ps("QB", [128, 512], F32)
    SBk = [c.ps("SBk%d" % i, [128, 4, 128], F32) for i in range(2)]

    S.op("dve", lambda E: E.memset(vm[:, :, :, 128:129], 1.0), writes=[rvm])
    for mc in range(2):
        c.load("sp", xt[:], mem[mc * 128:(mc + 1) * 128, :], rx)
        rmsnorm_tile(c, xt, rx, gA, rgA, u, ru, junk, rj, ss, rss)
        transpose_to(c, u, ru, 16, lambda k0, n, mc=mc: memnT[:, k0:k0 + n, mc * 128:(mc + 1) * 128], rmT, TB, rTB, idt, rid)
    for mc in range(2):
        for kc in range(16):
            S.op("pe", lambda E, kc=kc, mc=mc: E.matmul(QB[:], lhsT=memnT[:, kc, mc * 128:(mc + 1) * 128], rhs=wkv[:, kc, :],
                                                        start=(kc == 0), stop=(kc == 15)), reads=[rmT, rwkv], writes=[rQB])
        S.op("act", lambda E: E.activation(out=kq[:], in_=QB[:], func=AF.Copy), reads=[rQB], writes=[rkq])
        transpose_to(c, kq, rkq, 4, lambda k0, n, mc=mc: kmT[:, k0:k0 + n, mc * 128:(mc + 1) * 128], rkmT, TB, rTB, idt, rid)
    c.load("pool", wkv[:], wv_d.rearrange("(kc p) n -> p kc n", p=128), rwkv)
    for mc in range(2):
        for kc in range(16):
            S.op("pe", lambda E, kc=kc, mc=mc: E.matmul(QB[:], lhsT=memnT[:, kc, mc * 128:(mc + 1) * 128], rhs=wkv[:, kc, :],
                                                        start=(kc == 0), stop=(kc == 15)), reads=[rmT, rwkv], writes=[rQB])
        S.op("act", lambda E, mc=mc: E.activation(out=vm[:, mc, :, 0:128], in_=QB[:].rearrange("p (h d) -> p h d", h=4), func=AF.Copy),
             reads=[rQB], writes=[rvm])
    c.load("sp", gA[:], g_q.partition_broadcast(128), rgA)

    if mode == "pool":
        c.load("sp", xt[:], halo, rx)
        rmsnorm_tile(c, xt, rx, gm, rgm, us[1][0], us[1][1], junk, rj, ss, rss)

    for tt in range(ntile):
        c.load("sp", xt[:], xin[tt * 128:(tt + 1) * 128, :], rx)
        if mode == "nsa":
            o_sb, ro = o_sbs[tt % 2]
            c.load("sp", o_sb[:], o_d[tt * 128:(tt + 1) * 128, :], ro)
            transpose_to(c, o_sb, ro, 16, lambda k0, n: aT[:, k0:k0 + n, :], raT, TB, rTB, idt, rid)
            for cb in range(4):
                acc, racc = A[cb]
                for kc in range(16):
                    S.op("pe", lambda E, kc=kc, cb=cb, acc=acc: E.matmul(acc[:], lhsT=aT[:, kc, :], rhs=wbig[:, kc, cb * 512:(cb + 1) * 512],
                                                                        start=(kc == 0), stop=(kc == 15)), reads=[raT, rwbig], writes=[racc])
                S.op("dve", lambda E, cb=cb, acc=acc: E.tensor_tensor(out=h1[:, cb * 512:(cb + 1) * 512], in0=acc[:], in1=xt[:, cb * 512:(cb + 1) * 512], op=ALU.add),
                     reads=[racc, rx], writes=[rh1])
        else:
            ucur, rucur = us[tt % 2]
            uprev, ruprev = us[(tt + 1) % 2]
            rmsnorm_tile(c, xt, rx, gm, rgm, ucur, rucur, junk, rj, ss, rss)
            bsel = 0 if tt == 0 else 1
            for gi in range(4):
                for cc in range(4):
                    fc = gi * 4 + cc
                    S.op("pe", lambda E, fc=fc, cc=cc, gi=gi, ucur=ucur, bsel=bsel: E.matmul(QB[:, cc * 128:(cc + 1) * 128], lhsT=ucur[:, fc * 128:(fc + 1) * 128],
                                                                                rhs=Bm[:, bsel, gi, :], start=True, stop=False), reads=[rucur, rBm], writes=[rQB])
                    S.op("pe", lambda E, fc=fc, cc=cc, gi=gi, uprev=uprev: E.matmul(QB[:, cc * 128:(cc + 1) * 128], lhsT=uprev[:, fc * 128:(fc + 1) * 128],
                                                                                  rhs=Bm[:, 2, gi, :], start=False, stop=True), reads=[ruprev, rBm], writes=[rQB])
                S.op("act", lambda E, gi=gi: E.activation(out=dT[:, gi * 4:gi * 4 + 4, :], in_=QB[:].rearrange("p (c t) -> p c t", c=4), func=AF.Copy),
                     reads=[rQB], writes=[rdT])
            for gi in range(4):
                acc, racc = A[gi]
                for cc in range(4):
                    S.op("pe", lambda E, gi=gi, cc=cc, acc=acc: E.matmul(acc[:], lhsT=dT[:, gi * 4 + cc, :], rhs=wp[:, gi, cc, :], start=(cc == 0), stop=(cc == 3)),
                         reads=[rdT, rwp], writes=[racc])
                sl = slice(gi * 512, (gi + 1) * 512)
                S.op("dve", lambda E, acc=acc, sl=sl: E.tensor_tensor(out=zt[:], in0=acc[:], in1=bt[:, sl], op=ALU.add), reads=[racc, rbt], writes=[rzt])
                S.op("dve", lambda E, sl=sl: E.tensor_tensor(out=zt[:], in0=zt[:], in1=sct[:, sl], op=ALU.mult), reads=[rzt, rsct], writes=[rzt])
                S.op("dve", lambda E, sl=sl: E.tensor_tensor(out=h1[:, sl], in0=zt[:], in1=xt[:, sl], op=ALU.add), reads=[rzt, rx], writes=[rh1])
        rmsnorm_tile(c, h1, rh1, gA, rgA, u, ru, junk, rj, ss, rss)
        transpose_to(c, u, ru, 16, lambda k0, n: aT[:, k0:k0 + n, :], raT, TB, rTB, idt, rid)
        for kc in range(16):
            S.op("pe", lambda E, kc=kc: E.matmul(QB[:], lhsT=aT[:, kc, :], rhs=wq[:, kc, :], start=(kc == 0), stop=(kc == 15)),
                 reads=[raT, rwq], writes=[rQB])
        S.op("act", lambda E: E.activation(out=kq[:], in_=QB[:], func=AF.Copy, scale=128.0 ** -0.5), reads=[rQB], writes=[rkq])
        transpose_to(c, kq, rkq, 4, lambda k0, n: qmT[:, k0:k0 + n, :], rqmT, TB, rTB, idt, rid)
        for mc in range(2):
            sb, rsb = SBk[mc]
            for h in range(4):
                S.op("pe", lambda E, h=h, mc=mc, sb=sb: E.matmul(sb[:, h, :], lhsT=kmT[:, h, mc * 128:(mc + 1) * 128], rhs=qmT[:, h, :], start=True, stop=True),
                     reads=[rkmT, rqmT], writes=[rsb])
            S.op("act", lambda E, mc=mc, sb=sb: E.activation(out=pm[:, mc, :, :], in_=sb[:], func=AF.Exp), reads=[rsb], writes=[rpm])
        for h in range(4):
            acc, racc = A[h]
            for mc in range(2):
                S.op("pe", lambda E, h=h, mc=mc, acc=acc: E.matmul(acc[:, 0:129], lhsT=pm[:, mc, h, :], rhs=vm[:, mc, h, :], start=(mc == 0), stop=(mc == 1)),
                     reads=[rpm, rvm], writes=[racc])
            S.op("act" if h % 2 == 0 else "dve",
                 (lambda E, h=h, acc=acc: E.activation(out=om32[:, h, :], in_=acc[:, 0:129], func=AF.Copy)) if h % 2 == 0 else
                 (lambda E, h=h, acc=acc: E.tensor_copy(out=om32[:, h, :], in_=acc[:, 0:129])), reads=[racc], writes=[rom32])
        S.op("dve", lambda E: E.reciprocal(out=rdn[:], in_=om32[:, :, 128]), reads=[rom32], writes=[rrdn])
        S.op("dve", lambda E: E.tensor_tensor(out=om[:], in0=om32[:, :, 0:128], in1=rdn[:].unsqueeze(2).to_broadcast([128, 4, 128]), op=ALU.mult),
             reads=[rom32, rrdn], writes=[rom])
        transpose_to(c, om[:].rearrange("p h d -> p (h d)"), rom, 4, lambda k0, n: omT[:, k0:k0 + n, :], romT, TB, rTB, idt, rid)
        for cb in range(4):
            acc, racc = A[cb]
            for kc in range(4):
                S.op("pe", lambda E, kc=kc, cb=cb, acc=acc: E.matmul(acc[:], lhsT=omT[:, kc, :], rhs=wo[:, kc, cb * 512:(cb + 1) * 512], start=(kc == 0), stop=(kc == 3)),
                     reads=[romT, rwo], writes=[racc])
            S.op("dve", lambda E, cb=cb, acc=acc: E.tensor_tensor(out=ho[:, cb * 512:(cb + 1) * 512], in0=acc[:], in1=h1[:, cb * 512:(cb + 1) * 512], op=ALU.add),
                 reads=[racc, rh1], writes=[rho])
        c.store("sp", hout[tt * 128:(tt + 1) * 128, :], ho[:], rho)
    return c.finish()


def pool_bmats(first):
    B = np.zeros((128, 3, 4, 128), np.float32)
    s = np.arange(128)[:, None]
    t = np.arange(128)[None, :]
    for gi, w in enumerate((2, 4, 8, 16)):
        gen = ((s > t - w) & (s <= t)) / float(w) - (s == t)
        cnt = np.minimum(t + 1, w).astype(np.float32)
        fst = ((s > t - w) & (s <= t)) / cnt - (s == t)
        B[:, 0, gi] = fst if first else gen
        B[:, 1, gi] = gen
        B[:, 2, gi] = ((s - 128) > (t - w)) / float(w)
    return B.astype(NPBF)


DFF = 5632
import os as _os
KF_DBG = int(_os.environ.get('KF_DBG', '0'))
KF_SCR_MIN = int(_os.environ.get('KF_SCR_MIN', '8'))


def build_kf(mode, ntile=16, c=None):
    c = c or Ctx()
    S = c.S
    T = ntile * 128
    nst = ntile // 4
    hin = c.din("hin", [T, D], F32)
    g = c.din("g", [D], F32)
    wg_d = c.din("wg", [D, DFF], F32)
    wu_d = c.din("wu", [D, DFF], F32)
    wd_d = c.din("wd", [DFF, D], F32)
    identf = c.din("identf", [128, 128], F32)
    y_d = c.dout("y", [T, D], F32)
    moe = mode == "moe"
    if moe:
        onehot_d = c.din("onehot", [128, 8], F32)
        oh, roh = c.sb("oh", [128, 8], F32)
        c.load("sp", oh[:], onehot_d, roh)
        routerT_d = c.din("routerT", [8, D], F32)
        rtbs = [c.sb("rtb%d" % i, [128, D], F32) for i in range(2)]
        j32s = [c.sb("j32_%d" % i, [128, D], F32) for i in range(2)]
        lg, rlg = c.sb("lg", [128, 8], F32)
        tm, rtm = c.sb("tm", [128, 8], F32)
        sm, rsm = c.sb("sm", [128, 8], F32)

    idf, ridf = c.sb("idf", [128, 128], F32)
    c.load("sp", idf[:], identf, ridf)
    gt, rgt = c.sb("gt", [128, D], F32)
    c.load("sp", gt[:], g.partition_broadcast(128), rgt)
    xt, rx = c.sb("xt", [128, D], F32)
    junk, rj = c.sb("junk", [128, D], BF16)
    ss, rss = c.sb("ss", [128, 2], F32)
    u32, ru32 = c.sb("u32", [128, D], F32)
    uTs = [c.sb("uT%d" % i, [128, 16, 512], BF16) for i in range(2 if nst > 1 else 1)] * (1 if nst > 1 else 2)
    hT, rhT = c.sb("hT", [128, 22, 512], BF16)
    wgs = [c.sb("wg%d" % i, [128, 16, 256], BF16) for i in range(2)]
    wus = [c.sb("wu%d" % i, [128, 16, 256], BF16) for i in range(2)]
    wds = [c.sb("wd%d" % i, [128, 22, 256], BF16) for i in range(2)]
    ys = [c.sb("y%d" % i, [128, D], F32) for i in range(4)]
    sgs = [c.sb("sg%d" % i, [128, 512], F32) for i in range(2)]
    gwss = [c.sb("gws%d" % i, [128, 4], F32) for i in range(2)]

    TB, rTB = c.ps("TB32", [128, 4, 128], F32)
    GB = [c.ps("GB%d" % i, [128, 512], F32) for i in range(2)]
    UB = [c.ps("UB%d" % i, [128, 512], F32) for i in range(2)]
    AB = [c.ps("AB%d" % i, [128, 256], F32) for i in range(2)]
    LB, rLB = c.ps("LB", [128, 8], F32)

    if not moe or KF_DBG >= 1:
        for gws, rgws in gwss:
            S.op("dve", lambda E, gws=gws: E.memset(gws[:], 1.0), writes=[rgws])
    wi = 0
    wdi = 0
    use_scr = nst >= KF_SCR_MIN
    rscr = Res()
    if use_scr:
        wg_s = c.nc.dram_tensor("wg_s", [2, 22, 128, 16 * 256], BF16).ap()
        wd_s = c.nc.dram_tensor("wd_s", [2, 8, 128, 22 * 256], BF16).ap()
        for fbg in range(22):
            f0 = fbg * 256
            wg, rwg = wgs[fbg % 2]
            wu, rwu = wus[fbg % 2]
            c.load("pool", wg[:], wg_d[:, f0:f0 + 256].rearrange("(kc p) n -> p kc n", p=128), rwg)
            S.dma("sp", wg_s[0, fbg], wg[:].rearrange("p k n -> p (k n)"), reads=[rwg], writes=[rscr])
            c.load("pool", wu[:], wu_d[:, f0:f0 + 256].rearrange("(kc p) n -> p kc n", p=128), rwu)
            S.dma("sp", wg_s[1, fbg], wu[:].rearrange("p k n -> p (k n)"), reads=[rwu], writes=[rscr])
        for half in range(2):
            for db in range(8):
                wd, rwd = wds[(half * 8 + db) % len(wds)]
                c.load("pool", wd[:], wd_d[half * 2816:(half + 1) * 2816, db * 256:(db + 1) * 256].rearrange("(j p) n -> p j n", p=128), rwd)
                S.dma("sp", wd_s[half, db], wd[:].rearrange("p j n -> p (j n)"), reads=[rwd], writes=[rscr])
    def phaseA_tile(st, tt):
        uT, ruT = uTs[st % 2]
        gws, rgws = gwss[st % 2]
        row = (st * 4 + tt) * 128
        c.load("sp", xt[:], hin[row:row + 128, :], rx)
        rmsnorm_tile(c, xt, rx, gt, rgt, u32, ru32, junk, rj, ss, rss)
        for k0 in range(0, 16, 4):
            for k in range(4):
                S.op("pe", lambda E, k=k, k0=k0: E.transpose(out=TB[:, k, :], in_=u32[:, (k0 + k) * 128:(k0 + k + 1) * 128], identity=idf[:]),
                     reads=[ru32, ridf], writes=[rTB])
            S.op("act", lambda E, k0=k0, tt=tt: E.activation(out=uT[:, k0:k0 + 4, tt * 128:(tt + 1) * 128], in_=TB[:], func=AF.Copy), reads=[rTB], writes=[ruT])
        if moe and KF_DBG < 1:
            for ex in range(8):
                rb_, rrb_ = rtbs[ex % 2]
                c.load("sp", rb_[:], routerT_d[ex].partition_broadcast(128), rrb_)
                jb, rjb = j32s[ex % 2]
                S.op("dve", lambda E, rb_=rb_, jb=jb: E.tensor_tensor(out=jb[:], in0=u32[:], in1=rb_[:], op=ALU.mult), reads=[ru32, rrb_], writes=[rjb])
                S.op("act", lambda E, ex=ex, jb=jb: E.activation(out=jb[:], in_=jb[:], func=AF.Copy, accum_out=lg[:, ex:ex + 1]), reads=[rjb], writes=[rjb, rlg])
            S.op("dve", lambda E: E.tensor_reduce(out=sm[:, 0:1], in_=lg[:], axis=AX.X, op=ALU.max), reads=[rlg], writes=[rsm])
            S.op("dve", lambda E: E.tensor_scalar(out=tm[:], in0=lg[:], scalar1=sm[:, 0:1], scalar2=-1e30, op0=ALU.is_ge, op1=ALU.mult), reads=[rlg, rsm], writes=[rtm])
            S.op("dve", lambda E: E.tensor_tensor(out=tm[:], in0=tm[:], in1=lg[:], op=ALU.add), reads=[rtm, rlg], writes=[rtm])
            S.op("dve", lambda E: E.tensor_reduce(out=sm[:, 1:2], in_=tm[:], axis=AX.X, op=ALU.max), reads=[rtm], writes=[rsm])
            S.op("dve", lambda E: E.tensor_tensor(out=tm[:], in0=lg[:], in1=oh[:], op=ALU.mult), reads=[rlg, roh], writes=[rtm])
            S.op("dve", lambda E: E.tensor_reduce(out=sm[:, 2:3], in_=tm[:], axis=AX.X, op=ALU.add), reads=[rtm], writes=[rsm])
            S.op("dve", lambda E: E.tensor_scalar(out=sm[:, 3:5], in0=sm[:, 1:3], scalar1=sm[:, 0:1], scalar2=None, op0=ALU.subtract), reads=[rsm], writes=[rsm])
            S.op("act", lambda E: E.activation(out=sm[:, 3:5], in_=sm[:, 3:5], func=AF.Exp), reads=[rsm], writes=[rsm])
            S.op("dve", lambda E: E.tensor_scalar(out=sm[:, 3:4], in0=sm[:, 3:4], scalar1=1.0, scalar2=None, op0=ALU.add), reads=[rsm], writes=[rsm])
            S.op("dve", lambda E: E.reciprocal(out=sm[:, 3:4], in_=sm[:, 3:4]), reads=[rsm], writes=[rsm])
            S.op("dve", lambda E: E.tensor_tensor(out=sm[:, 5:6], in0=sm[:, 2:3], in1=sm[:, 1:2], op=ALU.is_ge), reads=[rsm], writes=[rsm])
            S.op("dve", lambda E: E.tensor_tensor(out=sm[:, 4:5], in0=sm[:, 4:5], in1=sm[:, 3:4], op=ALU.mult), reads=[rsm], writes=[rsm])
            S.op("dve", lambda E, tt=tt: E.tensor_tensor(out=gws[:, tt:tt + 1], in0=sm[:, 4:5], in1=sm[:, 5:6], op=ALU.mult), reads=[rsm], writes=[rgws])

    for tt in range(4):
        phaseA_tile(0, tt)
    for st in range(nst):
        uT, ruT = uTs[st % 2]
        gws, rgws = gwss[st % 2]
        for tt in range(4):
            row = (st * 4 + tt) * 128
            y_sb, ry = ys[tt]
            if moe:
                S.op("dve", lambda E, y_sb=y_sb: E.memset(y_sb[:], 0.0), writes=[ry])
            else:
                c.load("sp", y_sb[:], hin[row:row + 128, :], ry)
        for half in range(2):
            for fb in range(11):
                f0 = half * 2816 + fb * 256
                wg, rwg = wgs[wi % 2]
                wu, rwu = wus[wi % 2]
                wi += 1
                if use_scr:
                    S.dma("pool", wg[:].rearrange("p k n -> p (k n)"), wg_s[0, half * 11 + fb], reads=[rscr], writes=[rwg])
                    S.dma("pool", wu[:].rearrange("p k n -> p (k n)"), wg_s[1, half * 11 + fb], reads=[rscr], writes=[rwu])
                else:
                    c.load("pool", wg[:], wg_d[:, f0:f0 + 256].rearrange("(kc p) n -> p kc n", p=128), rwg)
                    c.load("pool", wu[:], wu_d[:, f0:f0 + 256].rearrange("(kc p) n -> p kc n", p=128), rwu)
                for j in range(2):
                    gb, rgb = GB[j]
                    ub, rub = UB[j]
                    sg, rsg = sgs[j]
                    for kc in range(16):
                        S.op("pe", lambda E, kc=kc, j=j, gb=gb, wg=wg, uT=uT: E.matmul(gb[:], lhsT=wg[:, kc, j * 128:(j + 1) * 128], rhs=uT[:, kc, :],
                                                                              start=(kc == 0), stop=(kc == 15)), reads=[rwg, ruT], writes=[rgb])
                    for kc in range(16):
                        S.op("pe", lambda E, kc=kc, j=j, ub=ub, wu=wu, uT=uT: E.matmul(ub[:], lhsT=wu[:, kc, j * 128:(j + 1) * 128], rhs=uT[:, kc, :],
                                                                              start=(kc == 0), stop=(kc == 15)), reads=[rwu, ruT], writes=[rub])
                    S.op("act", lambda E, sg=sg, gb=gb: E.activation(out=sg[:], in_=gb[:], func=AF.Silu), reads=[rgb], writes=[rsg])
                    S.op("dve", lambda E, sg=sg, ub=ub, fb=fb, j=j: E.tensor_tensor(out=hT[:, fb * 2 + j, :], in0=sg[:], in1=ub[:], op=ALU.mult),
                         reads=[rsg, rub], writes=[rhT])
                if half == 0 and st + 1 < nst and fb in (1, 3, 5, 7):
                    phaseA_tile(st + 1, (fb - 1) // 2)
            for db in range(8):
                wd, rwd = wds[wdi % len(wds)]
                wdi += 1
                if use_scr:
                    S.dma("pool", wd[:].rearrange("p j n -> p (j n)"), wd_s[half, db], reads=[rscr], writes=[rwd])
                else:
                    c.load("pool", wd[:], wd_d[half * 2816:(half + 1) * 2816, db * 256:(db + 1) * 256].rearrange("(j p) n -> p j n", p=128), rwd)
                for tt in range(4):
                    ab, rab = AB[tt % 2]
                    y_sb, ry = ys[tt]
                    for j in range(22):
                        S.op("pe", lambda E, j=j, tt=tt, ab=ab, wd=wd: E.matmul(ab[:], lhsT=hT[:, j, tt * 128:(tt + 1) * 128], rhs=wd[:, j, :],
                                                                              start=(j == 0), stop=(j == 21)), reads=[rhT, rwd], writes=[rab])
                    S.op("dve", lambda E, ab=ab, y_sb=y_sb, tt=tt, db=db, gws=gws: E.scalar_tensor_tensor(
                        out=y_sb[:, db * 256:(db + 1) * 256], in0=ab[:], scalar=gws[:, tt:tt + 1], in1=y_sb[:, db * 256:(db + 1) * 256],
                        op0=ALU.mult, op1=ALU.add), reads=[rab, rgws, ry], writes=[ry])
        for tt in range(4):
            row = (st * 4 + tt) * 128
            y_sb, ry = ys[tt]
            c.store("sp", y_d[row:row + 128, :], y_sb[:], ry)
    return c.finish()


def build_kz(ntile=16):
    c = Ctx()
    S = c.S
    T = ntile * 128
    h_d = c.din("h", [T, D], F32)
    parts = c.din("parts", [8, T, D], F32)
    g = c.din("g", [D], F32)
    out = c.dout("out", [T, D], F32)
    gt, rgt = c.sb("gt", [128, D], F32)
    c.load("sp", gt[:], g.partition_broadcast(128), rgt)
    accs = [c.sb("acc%d" % i, [128, D], F32) for i in range(2)]
    pts = [c.sb("pt%d" % i, [128, D], F32) for i in range(3)]
    junk, rj = c.sb("junk", [128, D], BF16)
    ss, rss = c.sb("ss", [128, 2], F32)
    outs = [c.sb("o%d" % i, [128, D], F32) for i in range(2)]
    k = 0
    for tt in range(ntile):
        acc, racc = accs[tt % 2]
        o_sb, ro = outs[tt % 2]
        c.load("sp", acc[:], h_d[tt * 128:(tt + 1) * 128, :], racc)
        for e in range(8):
            pt, rpt = pts[k % 3]
            k += 1
            c.load("sp", pt[:], parts[e, tt * 128:(tt + 1) * 128, :], rpt)
            S.op("dve", lambda E, acc=acc, pt=pt: E.tensor_tensor(out=acc[:], in0=acc[:], in1=pt[:], op=ALU.add), reads=[racc, rpt], writes=[racc])
        rmsnorm_tile(c, acc, racc, gt, rgt, o_sb, ro, junk, rj, ss, rss)
        c.store("sp", out[tt * 128:(tt + 1) * 128, :], o_sb[:], ro)
    return c.finish()


def build_kbf(ntile=16):
    c = Ctx()
    c.chained = True
    hmid = c.scratch("hmid", [ntile * 128, D], F32)
    c.prefix = "a_"
    c.bind = {"hout": hmid}
    build_kb("nsa", ntile, c=c)
    c.next_phase("b_", {"hin": hmid})
    build_kf("dense", ntile, c=c)
    c.chained = False
    return c.finish()


def _run(nc, in_maps):
    res = run_bass_kernel_spmd(nc, in_maps, core_ids=list(range(NCORES)))
    et = getattr(res, "exec_time_ns", None)
    if et is not None:
        print("[launch exec_time_ns]", et, flush=True)
    return res.results


def kernel(**inp):
    f32 = np.float32
    x = np.ascontiguousarray(inp["x"][0], dtype=f32)
    ident = np.eye(128).astype(NPBF)
    identf = np.eye(128, dtype=f32)
    sl = lambda ci: slice(ci * TPC, (ci + 1) * TPC)
    in_maps = []
    for ci in range(NCORES):
        pos = np.arange(ci * TPC, (ci + 1) * TPC)
        cq, sq = rope_tables(pos, 128 ** -0.5)
        ck, sk = rope_tables(pos)
        in_maps.append({"x": x[sl(ci)], "g": inp["norm_mix"][0], "w": inp["nsa_w_in"][0], "gb": inp["nsa_gate_b"][0],
                        "ropet": np.stack([cq, sq, ck, sk]), "ident": ident})
    r1 = _run(build_k1(16), in_maps)
    qT_full = np.zeros((16, 128, SEQ), NPBF)
    feat = np.zeros((4, 4, 128, SEQ), NPBF)
    vs_full = np.zeros((SEQ, 4, 128), NPBF)
    vw_full = np.zeros((SEQ, 4, 128), NPBF)
    gates_full = np.zeros((SEQ, 48), f32)
    for ci in range(NCORES):
        fT = r1[ci]["fT"]
        qT_full[:, :, sl(ci)] = fT[0:4].reshape(16, 128, TPC)
        feat[:, :, :, sl(ci)] = fT[4:8]
        vs_full[sl(ci)] = r1[ci]["vtok"][0].reshape(TPC, 4, 128)
        vw_full[sl(ci)] = r1[ci]["vtok"][1].reshape(TPC, 4, 128)
        gates_full[sl(ci)] = r1[ci]["gates"]
    full = np.zeros((2, 4, 128, SEQ + 16), NPBF)
    full[0, :, :, :SEQ] = feat[0]
    full[1, :, :, :SEQ] = feat[1]
    w1 = np.stack([inp["nsa_cmp_k_w1"][0], inp["nsa_cmp_v_w1"][0]])
    w2 = np.stack([inp["nsa_cmp_k_w2"][0], inp["nsa_cmp_v_w2"][0]])
    peT = np.stack([np.ascontiguousarray(inp["nsa_pe_k"][0].T), np.ascontiguousarray(inp["nsa_pe_v"][0].T)])
    in_maps = []
    for ci in range(NCORES):
        cpos = (np.arange(128) + 128 * ci) * 16 + 31
        cc, sc = rope_tables(cpos)
        in_maps.append({"xT": np.ascontiguousarray(full[:, :, :, ci * TPC:ci * TPC + TPC + 16]), "w1": w1, "w2": w2, "peT": peT,
                        "ropec": np.stack([cc, sc]), "ident": ident})
    r2 = _run(build_k2(), in_maps)
    kcT_full = np.zeros((4, 128, 1024), NPBF)
    vc_full = np.zeros((1024, 4, 128), NPBF)
    for ci in range(NCORES):
        kcT_full[:, :, ci * 128:(ci + 1) * 128] = r2[ci]["kcT"]
        vc_full[ci * 128:(ci + 1) * 128] = r2[ci]["vc"]
    E, W = k3_consts()
    ksT_full = np.ascontiguousarray(feat[2])
    kwT_full = np.ascontiguousarray(feat[3])
    in_maps = [k3_inputs(ci, 16, qT_full, ksT_full, vs_full, kcT_full, vc_full, kwT_full, vw_full, gates_full, (E, W, ident))
               for ci in range(NCORES)]
    r3 = _run(build_k3(16), in_maps)
    o_full = np.zeros((SEQ, D), NPBF)
    for ci in range(NCORES):
        for r in range(16):
            qb = 8 * r + ci
            o_full[128 * qb:128 * qb + 128] = r3[ci]["o"][r]
    def memw(L):
        return {"mem": inp["mem"][0], "g_kv": inp["norm_mem_kv"][L], "g_q": inp["norm_mem_q"][L], "wq": inp["mem_wq"][L],
                "wk": inp["mem_wk"][L], "wv": inp["mem_wv"][L], "wo": inp["mem_wo"][L], "ident": ident}
    in_maps = []
    for ci in range(NCORES):
        m = {("a_" + k): v for k, v in dict(memw(0), xin=x[sl(ci)], o=o_full[sl(ci)], w_out=inp["nsa_w_out"][0]).items()}
        m.update({"b_g": inp["norm_ffn"][0], "b_wg": inp["ffn_w_gate"][0], "b_wu": inp["ffn_w_up"][0],
                  "b_wd": inp["ffn_w_down"][0], "b_identf": identf})
        in_maps.append(m)
    r5 = _run(build_kbf(16), in_maps)
    h3 = np.concatenate([r5[ci]["b_y"] for ci in range(NCORES)], axis=0)
    in_maps = []
    for ci in range(NCORES):
        halo = h3[ci * TPC - 128:ci * TPC] if ci > 0 else np.zeros((128, D), f32)
        in_maps.append(dict(memw(1), xin=h3[sl(ci)], halo=halo, g_mix=inp["norm_mix"][1], Bm=pool_bmats(ci == 0),
                            wp=inp["pool_w"][0], bp=np.ascontiguousarray(inp["pool_b"][0].reshape(-1)), sc=inp["pool_scale"][0]))
    r6 = _run(build_kb("pool", 16), in_maps)
    h5 = np.concatenate([r6[ci]["hout"] for ci in range(NCORES)], axis=0)
    routerT = np.ascontiguousarray(inp["moe_router"][0].T)
    in_maps = []
    for ci in range(NCORES):
        oh = np.zeros((128, 8), f32)
        oh[:, ci] = 1.0
        in_maps.append({"hin": h5, "g": inp["norm_ffn"][1], "wg": inp["moe_w_gate"][0, ci], "wu": inp["moe_w_up"][0, ci],
                        "wd": inp["moe_w_down"][0, ci], "identf": identf, "routerT": routerT, "onehot": oh})
    r7 = _run(build_kf("moe", SEQ // 128), in_maps)
    in_maps = [{"h": h5[sl(ci)], "parts": np.stack([r7[e]["y"][sl(ci)] for e in range(NCORES)]), "g": inp["norm_final"]}
               for ci in range(NCORES)]
    r8 = _run(build_kz(16), in_maps)
    out = np.concatenate([r8[ci]["out"] for ci in range(NCORES)], axis=0)
    return out.reshape(1, SEQ, D).astype(f32)
```
